# Optimizing a Trainium2 kernel written in Bass

```python
import jax, jax.numpy as jnp
from jax import lax
import numpy as np

D_MODEL = 2048
BATCH = 4
SEQ = 2048
DEPTH = 1

HEAD_DIM = 128
ATTN_WIDTH = D_MODEL // 2
ATTN_HEADS = ATTN_WIDTH // HEAD_DIM
CONV_WIDTH = D_MODEL - ATTN_WIDTH
CONV_GROUP_DIM = 128
CONV_GROUPS = CONV_WIDTH // CONV_GROUP_DIM
MIX_WIDTH = ATTN_WIDTH + CONV_WIDTH
IN_WIDTH = 3 * ATTN_WIDTH + 3 * CONV_WIDTH
CONV_K = 3
MOBA_BLOCK = 256
MOBA_TOPK = 3
Q_CHUNK = 16
N_EXPERTS = 256
TOP_K = 8
N_GROUPS = 8
TOPK_GROUPS = 4
EXPERT_DIM = 512
SHARED_DIM = 512
ROUTED_SCALE = 2.5
MOE_BLOCK = 128
EPS = 1e-6

kernel_name = "hybrid_moba_shortconv_moe_adaln"


def rms_norm(x, gain):
    xf = x.astype(jnp.float32)
    y = xf * lax.rsqrt(jnp.mean(xf * xf, axis=-1, keepdims=True) + EPS)
    return (y * gain.astype(jnp.float32)).astype(x.dtype)


def moba_attention(q, k, v):
    b, h, s, dh = q.shape
    nb = -(-s // MOBA_BLOCK)
    s_pad = nb * MOBA_BLOCK
    pad = ((0, 0), (0, 0), (0, s_pad - s), (0, 0))
    kb = jnp.pad(k, pad).reshape(b, h, nb, MOBA_BLOCK, dh)
    vb = jnp.pad(v, pad).reshape(b, h, nb, MOBA_BLOCK, dh)
    scale = dh ** -0.5
    k_mean = jnp.mean(kb.astype(jnp.float32), axis=3)
    gate = jnp.einsum('bhsd,bhnd->bhsn', q.astype(jnp.float32), k_mean)
    q_blk = jnp.arange(s) // MOBA_BLOCK
    past = jnp.arange(nb)[None, :] < q_blk[:, None]
    gate = jnp.where(past, gate, -jnp.inf)
    n_sel = min(MOBA_TOPK, nb)
    _, sel = lax.top_k(gate, n_sel)
    sel_valid = sel < q_blk[:, None]
    bi = jnp.arange(b)[:, None, None, None]
    hi = jnp.arange(h)[None, :, None, None]

    def chunk(c0):
        qc = lax.dynamic_slice_in_dim(q, c0, Q_CHUNK, axis=2)
        selc = lax.dynamic_slice_in_dim(sel, c0, Q_CHUNK, axis=2)
        validc = lax.dynamic_slice_in_dim(sel_valid, c0, Q_CHUNK, axis=2)
        own = c0 // MOBA_BLOCK
        k_own = lax.dynamic_index_in_dim(kb, own, axis=2, keepdims=False)
        v_own = lax.dynamic_index_in_dim(vb, own, axis=2, keepdims=False)
        q_pos = c0 + jnp.arange(Q_CHUNK)
        k_pos = own * MOBA_BLOCK + jnp.arange(MOBA_BLOCK)
        causal = k_pos[None, :] <= q_pos[:, None]
        s_own = jnp.einsum('bhqd,bhkd->bhqk', qc, k_own).astype(jnp.float32) * scale
        s_own = jnp.where(causal, s_own, -jnp.inf)
        k_sel = kb[bi, hi, selc]
        v_sel = vb[bi, hi, selc]
        s_sel = jnp.einsum('bhqd,bhqnkd->bhqnk', qc, k_sel).astype(jnp.float32) * scale
        s_sel = jnp.where(validc[..., None], s_sel, -jnp.inf)
        scores = jnp.concatenate([s_own, s_sel.reshape(b, h, Q_CHUNK, n_sel * MOBA_BLOCK)], axis=-1)
        p = jax.nn.softmax(scores, axis=-1)
        p_own = p[..., :MOBA_BLOCK].astype(v.dtype)
        p_sel = p[..., MOBA_BLOCK:].reshape(b, h, Q_CHUNK, n_sel, MOBA_BLOCK).astype(v.dtype)
        return (jnp.einsum('bhqk,bhkd->bhqd', p_own, v_own)
                + jnp.einsum('bhqnk,bhqnkd->bhqd', p_sel, v_sel))

    n_chunks = s // Q_CHUNK
    outs = lax.map(chunk, jnp.arange(n_chunks) * Q_CHUNK)
    return outs.transpose(1, 2, 0, 3, 4).reshape(b, h, s, dh)


def short_conv_mixer(u, b_gate, c_gate, conv_w):
    z = c_gate * u
    y = lax.conv_general_dilated(z, conv_w[:, None, :].astype(z.dtype), window_strides=(1,),
                                 padding=[(CONV_K - 1, 0)],
                                 dimension_numbers=('NWC', 'WIO', 'NWC'),
                                 feature_group_count=z.shape[-1])
    return b_gate * y


def route(h, w_router, router_bias):
    n = h.shape[0]
    scores = jax.nn.sigmoid((h @ w_router).astype(jnp.float32))
    choice = scores + router_bias.astype(jnp.float32)
    grp = choice.reshape(n, N_GROUPS, N_EXPERTS // N_GROUPS)
    grp_score = jnp.sum(lax.top_k(grp, 2)[0], axis=-1)
    _, top_g = lax.top_k(grp_score, TOPK_GROUPS)
    gmask = jnp.any(top_g[..., None] == jnp.arange(N_GROUPS), axis=-2)
    emask = jnp.repeat(gmask, N_EXPERTS // N_GROUPS, axis=-1)
    _, idx = lax.top_k(jnp.where(emask, choice, -jnp.inf), TOP_K)
    w = jnp.take_along_axis(scores, idx, axis=-1)
    w = w / jnp.sum(w, axis=-1, keepdims=True) * ROUTED_SCALE
    return idx, w


def routed_experts(h, idx, w, w_gate, w_up, w_down):
    n, d = h.shape
    e = w_gate.shape[0]
    nk = n * TOP_K
    flat_e = idx.reshape(-1)
    order = jnp.argsort(flat_e)
    e_sorted = flat_e[order]
    tok_sorted = (order // TOP_K).astype(jnp.int32)
    w_sorted = w.reshape(-1)[order]
    counts = jnp.bincount(flat_e, length=e)
    blocks_per_e = (counts + MOBA_BLOCK * 0 + MOE_BLOCK - 1) // MOE_BLOCK
    blk_end = jnp.cumsum(blocks_per_e)
    blk_start = blk_end - blocks_per_e
    grp_start = jnp.cumsum(counts) - counts
    slot = blk_start[e_sorted] * MOE_BLOCK + (jnp.arange(nk) - grp_start[e_sorted])
    n_blocks = -(-nk // MOE_BLOCK) + e
    slot_tok = jnp.full((n_blocks * MOE_BLOCK,), n, jnp.int32).at[slot].set(tok_sorted)
    slot_w = jnp.zeros((n_blocks * MOE_BLOCK,), jnp.float32).at[slot].set(w_sorted)
    blk_expert = jnp.minimum(jnp.searchsorted(blk_end, jnp.arange(n_blocks), side='right'), e - 1)
    h_pad = jnp.concatenate([h, jnp.zeros((1, d), h.dtype)], axis=0)

    def step(acc, xs):
        toks, ws, ex = xs
        xb = h_pad[toks]
        hid = jax.nn.silu(xb @ w_gate[ex]) * (xb @ w_up[ex])
        yb = (hid @ w_down[ex]).astype(jnp.float32) * ws[:, None]
        return acc.at[toks].add(yb), None

    acc, _ = lax.scan(step, jnp.zeros((n + 1, d), jnp.float32),
                      (slot_tok.reshape(n_blocks, MOE_BLOCK), slot_w.reshape(n_blocks, MOE_BLOCK), blk_expert))
    return acc[:n].astype(h.dtype)


def setup_inputs(seed: int = 0) -> dict:
    key = jax.random.key(seed)
    ks = jax.random.split(key, 24)
    f32 = jnp.float32
    nrm = lambda k, shape, s: jax.random.normal(k, shape, f32) * s
    L, D = DEPTH, D_MODEL
    return {
        "x": nrm(ks[0], (BATCH, SEQ, D), 1.0),
        "c": nrm(ks[1], (BATCH, D), 1.0),
        "w_ada": nrm(ks[2], (L, D, 6 * D), 0.5 * D ** -0.5),
        "b_ada": nrm(ks[3], (L, 6 * D), 0.01),
        "norm1_g": 1.0 + nrm(ks[4], (L, D), 0.02),
        "w_in": nrm(ks[5], (L, D, IN_WIDTH), D ** -0.5),
        "q_norm_g": 1.0 + nrm(ks[6], (L, HEAD_DIM), 0.02),
        "k_norm_g": 1.0 + nrm(ks[7], (L, HEAD_DIM), 0.02),
        "conv_w": nrm(ks[8], (L, CONV_K, CONV_WIDTH), CONV_K ** -0.5),
        "attn_out_g": 1.0 + nrm(ks[9], (L, ATTN_WIDTH), 0.02),
        "conv_out_g": 1.0 + nrm(ks[10], (L, CONV_WIDTH), 0.02),
        "w_o": nrm(ks[11], (L, MIX_WIDTH, D), MIX_WIDTH ** -0.5),
        "norm2_g": 1.0 + nrm(ks[12], (L, D), 0.02),
        "w_router": nrm(ks[13], (L, D, N_EXPERTS), D ** -0.5),
        "router_bias": nrm(ks[14], (L, N_EXPERTS), 0.01),
        "w_gate": nrm(ks[15], (L, N_EXPERTS, D, EXPERT_DIM), D ** -0.5),
        "w_up": nrm(ks[16], (L, N_EXPERTS, D, EXPERT_DIM), D ** -0.5),
        "w_down": nrm(ks[17], (L, N_EXPERTS, EXPERT_DIM, D), EXPERT_DIM ** -0.5),
        "ws_gate": nrm(ks[18], (L, D, SHARED_DIM), D ** -0.5),
        "ws_up": nrm(ks[19], (L, D, SHARED_DIM), D ** -0.5),
        "ws_down": nrm(ks[20], (L, SHARED_DIM, D), SHARED_DIM ** -0.5),
    }


def reference(x, c, w_ada, b_ada, norm1_g, w_in, q_norm_g, k_norm_g, conv_w, attn_out_g,
              conv_out_g, w_o, norm2_g, w_router, router_bias, w_gate, w_up, w_down,
              ws_gate, ws_up, ws_down):
    b, s, d = x.shape
    A, Cw = ATTN_WIDTH, CONV_WIDTH
    for l in range(DEPTH):
        mod = (jax.nn.silu(c) @ w_ada[l] + b_ada[l])[:, None, :]
        sh1, sc1, g1, sh2, sc2, g2 = jnp.split(mod, 6, axis=-1)

        hmix = rms_norm(x, norm1_g[l]) * (1.0 + sc1) + sh1
        proj = hmix @ w_in[l]
        q, k, v, u, bg, cg = jnp.split(proj, [A, 2 * A, 3 * A, 3 * A + Cw, 3 * A + 2 * Cw], axis=-1)
        q = rms_norm(q.reshape(b, s, ATTN_HEADS, HEAD_DIM), q_norm_g[l]).transpose(0, 2, 1, 3)
        k = rms_norm(k.reshape(b, s, ATTN_HEADS, HEAD_DIM), k_norm_g[l]).transpose(0, 2, 1, 3)
        v = v.reshape(b, s, ATTN_HEADS, HEAD_DIM).transpose(0, 2, 1, 3)
        a = moba_attention(q, k, v)
        a = rms_norm(a, attn_out_g[l].reshape(ATTN_HEADS, 1, HEAD_DIM))
        a = a.transpose(0, 2, 1, 3).reshape(b, s, A)
        y = short_conv_mixer(u, bg, cg, conv_w[l])
        y = rms_norm(y.reshape(b, s, CONV_GROUPS, CONV_GROUP_DIM),
                     conv_out_g[l].reshape(CONV_GROUPS, CONV_GROUP_DIM)).reshape(b, s, Cw)
        mix = jnp.concatenate([a, y], axis=-1) @ w_o[l]
        x = x + g1 * mix

        hf = (rms_norm(x, norm2_g[l]) * (1.0 + sc2) + sh2).reshape(b * s, d)
        idx, wts = route(hf, w_router[l], router_bias[l])
        routed = routed_experts(hf, idx, wts, w_gate[l], w_up[l], w_down[l])
        shared = (jax.nn.silu(hf @ ws_gate[l]) * (hf @ ws_up[l])) @ ws_down[l]
        x = x + g2 * (routed + shared).reshape(b, s, d)
    return x
```

```python
from contextlib import ExitStack

import numpy as np
import ml_dtypes
import concourse.bass as bass
import concourse.mybir as mybir
from concourse.bass_utils import run_bass_kernel_spmd

F32 = mybir.dt.float32
BF16 = mybir.dt.bfloat16
AF = mybir.ActivationFunctionType
ALU = mybir.AluOpType
AX = mybir.AxisListType

NCORES = 8
D = 2048
S = 2048
NB = 4
NT = 1024
EPS = 1e-6
NEG = -30000.0


class _Eng:
    def __init__(self, name, sem):
        self.name = name
        self.sem = sem
        self.sig = 0
        self.pending = []
        self.waited = {}
        self.prog = []


class Tracker:
    ENG = ("pe", "act", "dve", "pool", "sp")

    def __init__(self, nc, stack):
        self.nc = nc
        self.stack = stack
        self.E = {n: _Eng(n, stack.enter_context(nc.semaphore("sem_" + n))) for n in self.ENG}
        self.dsem = {}
        self.last_w = {}
        self.readers = {}

    def _dsem(self, name):
        if name not in self.dsem:
            self.dsem[name] = [self.stack.enter_context(self.nc.semaphore("d_" + name)), 0]
        return self.dsem[name]

    def _deps(self, reads, writes):
        deps = []
        for k in reads:
            t = self.last_w.get(k)
            if t is not None:
                deps.append(t)
        for k in writes:
            t = self.last_w.get(k)
            if t is not None:
                deps.append(t)
            deps.extend(self.readers.get(k, ()))
        return deps

    def _wait(self, E, deps):
        for t in deps:
            if t[0] == "e":
                if t[1] == "pe" and E.name == "pe":
                    continue
                v = t[2]
                assert v is not None, "dependency on unsignaled op (%s)" % t[1]
                key = ("e", t[1])
                sem = self.E[t[1]].sem
            else:
                v = t[2]
                key = ("d", t[1])
                sem = self.dsem[t[1]][0]
            if E.waited.get(key, 0) >= v:
                continue
            E.waited[key] = v
            E.prog.append(("wait", sem, v))

    def _record(self, tok, reads, writes):
        for k in reads:
            self.readers.setdefault(k, []).append(tok)
        for k in writes:
            self.last_w[k] = tok
            self.readers[k] = []

    def op(self, en, fn, reads=(), writes=(), sig=True):
        E = self.E[en]
        self._wait(E, self._deps(reads, writes))
        tok = ["e", en, None]
        if sig:
            E.sig += 1
            tok[2] = E.sig
            for p in E.pending:
                p[2] = E.sig
            E.pending = []
        else:
            E.pending.append(tok)
        E.prog.append(("op", fn, sig))
        self._record(tok, reads, writes)

    def dma(self, q, out, in_, reads=(), writes=(), dsem="misc"):
        E = self.E[q]
        self._wait(E, self._deps(reads, writes))
        Dm = self._dsem(dsem)
        Dm[1] += 16
        tok = ["d", dsem, Dm[1]]
        E.prog.append(("dma", out, in_, Dm[0]))
        self._record(tok, reads, writes)

    def barrier(self):
        for E in self.E.values():
            assert not E.pending, "pending unsignaled ops on %s at barrier" % E.name
        for E in self.E.values():
            for O in self.E.values():
                if O is E or O.sig == 0:
                    continue
                if E.waited.get(("e", O.name), 0) < O.sig:
                    E.waited[("e", O.name)] = O.sig
                    E.prog.append(("wait", O.sem, O.sig))
            for dn, Dm in self.dsem.items():
                if Dm[1] and E.waited.get(("d", dn), 0) < Dm[1]:
                    E.waited[("d", dn)] = Dm[1]
                    E.prog.append(("wait", Dm[0], Dm[1]))
        self.last_w = {}
        self.readers = {}

    def emit(self):
        nc = self.nc
        self.barrier()
        with nc.Block() as block:
            def run(E, eng):
                for it in E.prog:
                    if it[0] == "wait":
                        eng.wait_ge(it[1], it[2])
                    elif it[0] == "op":
                        ins = it[1](eng)
                        if it[2]:
                            ins.then_inc(E.sem, 1)
                    else:
                        eng.dma_start(out=it[1], in_=it[2]).then_inc(it[3], 16)

            @block.tensor
            def _(eng):
                run(self.E["pe"], eng)

            @block.scalar
            def _(eng):
                run(self.E["act"], eng)

            @block.vector
            def _(eng):
                run(self.E["dve"], eng)

            @block.gpsimd
            def _(eng):
                run(self.E["pool"], eng)

            @block.sync
            def _(eng):
                run(self.E["sp"], eng)


def _wview(w2d):
    return w2d.rearrange("(kc p) n -> p kc n", p=128)


def build_l0():
    nc = bass.Bass("TRN2", target_bir_lowering=False)
    cT = nc.dram_tensor("cT", [128, 64], F32, kind="ExternalInput").ap()
    wa = nc.dram_tensor("wa", [D, 1536], F32, kind="ExternalInput").ap()
    ba = nc.dram_tensor("ba", [1, 1536], F32, kind="ExternalInput").ap()
    mod = nc.dram_tensor("mod", [4, 1536], F32, kind="ExternalOutput").ap()
    with ExitStack() as st:
        T = Tracker(nc, st)
        sb = lambda n, s, d: st.enter_context(nc.sbuf_tensor(n, s, d))
        c_f = sb("c_f", [128, 64], F32)
        c_b = sb("c_b", [128, 64], BF16)
        wt = sb("wt", [128, 16, 1536], BF16)
        bt = sb("bt", [4, 1536], F32)
        ot = sb("ot", [4, 1536], F32)
        ps = st.enter_context(nc.psum_tensor("ps", [128, 512], F32))
        T.dma("sp", c_f[:], cT, writes=["c_f"], dsem="a")
        T.dma("sp", bt[:], ba.partition_broadcast(4), writes=["bt"], dsem="b")
        T.dma("pool", wt[:], _wview(wa), writes=["wt"], dsem="w")
        T.op("act", lambda e: e.activation(out=c_b[:], in_=c_f[:], func=AF.Silu), reads=["c_f"], writes=["c_b"])
        for n in range(3):
            for kc in range(16):
                T.op("pe", lambda e, kc=kc, n=n: e.matmul(ps[0:4, :], lhsT=c_b[:, kc * 4:(kc + 1) * 4],
                                                          rhs=wt[:, kc, n * 512:(n + 1) * 512],
                                                          start=(kc == 0), stop=(kc == 15)),
                     reads=["c_b", "wt"], writes=["ps"], sig=(kc == 15))
            T.op("dve", lambda e, n=n: e.tensor_tensor(out=ot[:, n * 512:(n + 1) * 512], in0=ps[0:4, :],
                                                       in1=bt[:, n * 512:(n + 1) * 512], op=ALU.add),
                 reads=["ps", "bt"], writes=["ot"])
        T.dma("sp", mod, ot[:], reads=["ot"], writes=["mod"], dsem="o")
        T.emit()
    return nc


class Ctx:
    pass


def rms_T(T, C, src, src_key, gain, out_ap, out_key, n_feat):
    T.op("act", lambda e: e.activation(out=C.sqb[:], in_=src, func=AF.Square), reads=[src_key], writes=["sqb"])
    T.op("pe", lambda e: e.matmul(C.ps_n[:], lhsT=C.ones_b[:], rhs=C.sqb[:], start=True, stop=True),
         reads=["sqb"], writes=["ps_n"])
    T.op("act", lambda e: e.activation(out=C.rs[:], in_=C.ps_n[:], func=AF.Sqrt, bias=C.eps_t[:, 0:1], scale=1.0 / n_feat),
         reads=["ps_n"], writes=["rs"])
    T.op("dve", lambda e: e.reciprocal(out=C.rs[:], in_=C.rs[:]), reads=["rs"], writes=["rs"])
    T.op("dve", lambda e: e.scalar_tensor_tensor(out=out_ap, in0=src, scalar=gain, in1=C.rs[:], op0=ALU.mult, op1=ALU.mult),
         reads=[src_key, "rs"], writes=[out_key])


def norm_transpose(T, C, xt, xt_key, i, AT, shT, dst, tok0, tag):
    ssi = C.ss[:, i:i + 1]
    junk = C.junk
    T.op("dve", lambda e: e.memset(C.ss4[:], 0.0), writes=["ss4"])
    for q4 in range(4):
        T.op("act", lambda e, q4=q4: e.activation(out=junk[:, q4 * 512:(q4 + 1) * 512], in_=xt[:, q4 * 512:(q4 + 1) * 512], func=AF.Square,
                                                  accum_out=C.ss4[:, q4:q4 + 1]),
             reads=[xt_key, "ss4"], writes=["junk", "ss4"])
    T.op("dve", lambda e: e.tensor_reduce(out=ssi, in_=C.ss4[:], axis=AX.X, op=ALU.add), reads=["ss4"], writes=[("ss", tag, i)])
    T.op("act", lambda e: e.activation(out=ssi, in_=ssi, func=AF.Sqrt, bias=C.eps_t[:, 0:1], scale=1.0 / D),
         reads=[("ss", tag, i)], writes=[("ss", tag, i)])
    T.op("dve", lambda e: e.reciprocal(out=ssi, in_=ssi), reads=[("ss", tag, i)], writes=[("ss", tag, i)])
    xn = C.xn[i % 2]
    xk = "xn%d" % ((i % 2) if C.xn[0] is not C.xn[1] else 0)
    T.op("act", lambda e: e.activation(out=xn[:], in_=xt, func=AF.Copy, scale=ssi), reads=[xt_key, ("ss", tag, i)], writes=[xk])
    for g in range(4):
        pb = C.psb[g % 2]
        pk = "psb%d" % (g % 2)
        for j in range(4):
            dc = g * 4 + j
            T.op("pe", lambda e, dc=dc, j=j, pb=pb: e.transpose(out=pb[:, j * 128:(j + 1) * 128], in_=xn[:, dc * 128:(dc + 1) * 128],
                                                                identity=C.ident_b[:]),
                 reads=[xk, "ident_b"], writes=[pk], sig=(j == 3))
        for j in range(4):
            dc = g * 4 + j
            T.op("dve", lambda e, dc=dc, j=j, pb=pb: e.tensor_scalar(out=dst[:, dc, tok0:tok0 + 128], in0=pb[:, j * 128:(j + 1) * 128],
                                                                    scalar1=AT[:, dc:dc + 1], scalar2=shT[:, dc:dc + 1],
                                                                    op0=ALU.mult, op1=ALU.add),
                 reads=[pk, "modv"], writes=[(tag, dc, i)])


def expert_dense(T, C, hT, hkey, wg, wgk, wu, wuk, wd, wdk, epi):
    for th in range(2):
        for fc in range(4):
            for (w, wk, ps, pk) in ((wg, wgk, C.ps_g, "ps_g"), (wu, wuk, C.ps_u, "ps_u")):
                for kc in range(16):
                    T.op("pe", lambda e, w=w, ps=ps, kc=kc, fc=fc, th=th: e.matmul(
                        ps[:], lhsT=w[:, kc, fc * 128:(fc + 1) * 128], rhs=hT[:, kc, th * 512:(th + 1) * 512],
                        start=(kc == 0), stop=(kc == 15)),
                        reads=[wk, hkey], writes=[pk], sig=(kc == 15))
            T.op("act", lambda e: e.activation(out=C.sg[:], in_=C.ps_g[:], func=AF.Silu), reads=["ps_g"], writes=["sg"])
            T.op("dve", lambda e, fc=fc, th=th: e.tensor_tensor(out=C.hid[:, fc, th * 512:(th + 1) * 512], in0=C.sg[:], in1=C.ps_u[:],
                                                               op=ALU.mult),
                 reads=["sg", "ps_u"], writes=[("hid", fc, th)])
    cnt = 0
    for tt in range(8):
        for n in range(4):
            ps = C.ps_d[cnt % 2]
            pk = "ps_d%d" % (cnt % 2)
            cnt += 1
            for fc in range(4):
                T.op("pe", lambda e, ps=ps, fc=fc, tt=tt, n=n: e.matmul(
                    ps[:], lhsT=C.hid[:, fc, tt * 128:(tt + 1) * 128], rhs=wd[:, fc, n * 512:(n + 1) * 512],
                    start=(fc == 0), stop=(fc == 3)),
                    reads=[wdk, ("hid", fc, tt // 4)], writes=[pk], sig=(fc == 3))
            epi(tt, n, ps, pk)


def expert_dense2(T, C, hT, hkey, wg, wgk, wu, wuk, wd, wdk, g2bc, scal_of_tt, scal_key, x1, pbanks, cnt):
    unit = 0
    for th in range(2):
        for fc in range(4):
            for (w, wk, ps, pk) in ((wg, wgk, C.ps_g, "ps_g"), (wu, wuk, C.ps_u, "ps_u")):
                for kc in range(16):
                    T.op("pe", lambda e, w=w, ps=ps, kc=kc, fc=fc, th=th: e.matmul(
                        ps[:], lhsT=w[:, kc, fc * 128:(fc + 1) * 128], rhs=hT[:, kc, th * 512:(th + 1) * 512],
                        start=(kc == 0), stop=(kc == 15)),
                        reads=[wk, hkey], writes=[pk], sig=(kc == 15))
            T.op("act", lambda e: e.activation(out=C.sg[:], in_=C.ps_g[:], func=AF.Silu), reads=["ps_g"], writes=["sg"])
            T.op("dve", lambda e, fc=fc, th=th: e.tensor_tensor(out=C.hid[:, fc, th * 512:(th + 1) * 512], in0=C.sg[:], in1=C.ps_u[:],
                                                               op=ALU.mult),
                 reads=["sg", "ps_u"], writes=[("hid", fc, th)])
            if 2 <= unit < 6:
                j = unit - 2
                T.op("dve", lambda e, j=j: e.tensor_tensor(out=wd[:, j, :], in0=wd[:, j, :], in1=g2bc[:], op=ALU.mult),
                     reads=[wdk, "g2bc"], writes=[wdk])
            unit += 1
    for tt in range(8):
        scal = scal_of_tt(tt)
        for n in range(4):
            ps, pk = pbanks[cnt[0] % len(pbanks)]
            cnt[0] += 1
            for fc in range(4):
                T.op("pe", lambda e, ps=ps, fc=fc, tt=tt, n=n: e.matmul(
                    ps[:], lhsT=C.hid[:, fc, tt * 128:(tt + 1) * 128], rhs=wd[:, fc, n * 512:(n + 1) * 512],
                    start=(fc == 0), stop=(fc == 3)),
                    reads=[wdk, ("hid", fc, tt // 4)], writes=[pk], sig=(fc == 3))
            xs = x1[:, tt, n * 512:(n + 1) * 512]
            T.op("dve", lambda e, ps=ps, xs=xs, scal=scal: e.scalar_tensor_tensor(out=xs, in0=ps[:], scalar=scal, in1=xs,
                                                                                 op0=ALU.mult, op1=ALU.add),
                 reads=[pk, scal_key, ("x1", tt, n)], writes=[("x1", tt, n)])


def build_l1(debug=False):
    nc = bass.Bass("TRN2", target_bir_lowering=False)
    din = lambda n, s, d=F32: nc.dram_tensor(n, s, d, kind="ExternalInput").ap()
    xo = din("xo", [NT, D])
    xp = din("xp", [NT, D])
    modT = din("modT", [128, 96])
    g1row = din("g1row", [1, D])
    g2row = din("g2row", [1, D])
    ngT = din("ngT", [128, 32])
    w_in = din("w_in", [D, 6144])
    smallT = din("smallT", [128, 64])
    w_o = din("w_o", [D, D])
    w_r = din("w_r", [D, 256])
    rbias = din("rbias", [1, 256])
    wsg = din("wsg", [D, 512])
    wsu = din("wsu", [D, 512])
    wsd = din("wsd", [512, D])
    ident = din("ident", [128, 128])
    gbias = din("gbias", [128, 4 * 64])
    esel = din("esel", [64, 64 * 128])
    cmask = din("cmask", [128, 4 * 512])
    o_hfT = nc.dram_tensor("o_hfT", [D, NT], BF16, kind="ExternalOutput").ap()
    o_x1s = nc.dram_tensor("o_x1s", [NT, D], F32, kind="ExternalOutput").ap()
    o_rw = nc.dram_tensor("o_rw", [NT, 256], F32, kind="ExternalOutput").ap()

    with ExitStack() as st:
        T = Tracker(nc, st)
        C = Ctx()
        sb = lambda n, s, d, stack=st: stack.enter_context(nc.sbuf_tensor(n, s, d))
        psf = lambda n: st.enter_context(nc.psum_tensor(n, [128, 512], F32))
        C.ps_g = psf("ps_g"); C.ps_u = psf("ps_u"); C.ps_n = psf("ps_n")
        C.ps_d = [psf("ps_d0"), psf("ps_d1")]
        C.ps_x = psf("ps_x")
        C.psb = [st.enter_context(nc.psum_tensor("psb%d" % i, [128, 1024], BF16)) for i in range(2)]
        ident_f = sb("ident_f", [128, 128], F32)
        C.ident_b = sb("ident_b", [128, 128], BF16)
        C.ones_b = sb("ones_b", [128, 128], BF16)
        C.eps_t = sb("eps_t", [128, 1], F32)
        modv = sb("modv", [128, 96], F32)
        ng = sb("ng", [128, 32], F32)
        AT = sb("AT", [128, 32], F32)
        sm = sb("sm", [128, 64], F32)
        C.ss = sb("ss", [128, 32], F32)
        C.ss4 = sb("ss4", [128, 4], F32)
        C.sqb = sb("sqb", [128, 512], BF16)
        C.rs = sb("rs", [128, 512], F32)
        C.sg = sb("sg", [128, 512], F32)

        T.dma("sp", ident_f[:], ident, writes=["ident_f"], dsem="c0a")
        T.dma("sp", modv[:], modT, writes=["modv0"], dsem="c0b")
        T.dma("sp", ng[:], ngT, writes=["ng"], dsem="c0c")
        T.dma("sp", sm[:], smallT, writes=["sm"], dsem="c0d")
        T.op("dve", lambda e: e.tensor_copy(out=C.ident_b[:], in_=ident_f[:]), reads=["ident_f"], writes=["ident_b"])
        T.op("pool", lambda e: e.memset(C.ones_b[:], 1.0), writes=["ones_b"])
        T.op("pool", lambda e: e.memset(C.eps_t[:], EPS), writes=["eps_t"])
        T.op("dve", lambda e: e.tensor_scalar(out=AT[:, 0:16], in0=modv[:, 16:32], scalar1=1.0, scalar2=None, op0=ALU.add),
             reads=["modv0"], writes=["AT"])
        T.op("dve", lambda e: e.tensor_scalar(out=AT[:, 16:32], in0=modv[:, 64:80], scalar1=1.0, scalar2=None, op0=ALU.add),
             reads=["modv0"], writes=["AT"])
        T.op("dve", lambda e: e.tensor_tensor(out=AT[:], in0=AT[:], in1=ng[:], op=ALU.mult), reads=["AT", "ng"], writes=["AT"])
        T.barrier()
        A1T = AT[:, 0:16]; A2T = AT[:, 16:32]
        sh1T = modv[:, 0:16]; sh2T = modv[:, 48:64]
        qg = sm[:, 0:1]; kg = sm[:, 1:2]
        convw = lambda g, j: sm[:, 2 + g * 3 + j:3 + g * 3 + j]
        aog = lambda h: sm[:, 26 + h:27 + h]
        cog = lambda g: sm[:, 34 + g:35 + g]
        halo = sm[:, 42:43]

        yT = sb("yT", [128, 8, NT], BF16)
        aT = sb("aT", [128, 8, NT], BF16)
        with ExitStack() as sA:
            sbA = lambda n, s, d: sb(n, s, d, sA)
            QT = sbA("QT", [128, 8, NT], BF16)
            KT = sbA("KT", [128, 8, 2048], BF16)
            V = sbA("V", [128, 16, 1024], BF16)
            with ExitStack() as sP:
                sbP = lambda n, s, d: sb(n, s, d, sP)
                hT = sbP("hT", [128, 16, 2048], BF16)
                with ExitStack() as sX:
                    xb = [sb("xb%d" % i, [128, 2048], F32, sX) for i in range(2)]
                    C.junk = aT[:, 0:2, :].rearrange("p h t -> p (h t)")
                    _xn = sb("xn0", [128, 2048], BF16, sX)
                    C.xn = [_xn, _xn]
                    for i in range(16):
                        src = xp if i < 8 else xo
                        j = i % 8
                        T.dma("sp", xb[i % 2][:], src[j * 128:(j + 1) * 128, :], writes=["xb%d" % (i % 2)], dsem="x%d" % (i % 2))
                        norm_transpose(T, C, xb[i % 2][:], "xb%d" % (i % 2), i, A1T, sh1T, hT, i * 128, "hT")
                    T.barrier()
                wt = aT[:].rearrange("p h t -> p (h t)").rearrange("p (kc n) -> p kc n", n=512)
                if debug:
                    o_h = nc.dram_tensor("o_h", [128, 16 * 2048], BF16, kind="ExternalOutput").ap()
                    T.dma("sp", o_h, hT[:].rearrange("p c t -> p (c t)"), writes=["o_h"], dsem="dbg2")
                    T.barrier()
                uz = sbP("uz", [128, 8, NT + 2], BF16)
                zc = sbP("zc", [128, 512], F32)
                for n in (0, 1, 2, 3, 4, 5, 6, 7, 10, 11):
                    T.dma("pool", wt, _wview(w_in[:, n * 512:(n + 1) * 512]), writes=["wt"], dsem="wt")
                    kind = n // 2
                    for hh in range(4):
                        hd = (n % 2) * 4 + hh
                        if kind in (0, 1):
                            chunks = [(1024 + 512 * t, 512 * t) for t in range(2)] if kind == 0 else [(512 * t, 512 * t) for t in range(4)]
                            for (c0, o0) in chunks:
                                for kc in range(16):
                                    T.op("pe", lambda e, kc=kc, hh=hh, c0=c0: e.matmul(
                                        C.ps_x[:], lhsT=wt[:, kc, hh * 128:(hh + 1) * 128], rhs=hT[:, kc, c0:c0 + 512],
                                        start=(kc == 0), stop=(kc == 15)), reads=["wt"], writes=["ps_x"], sig=(kc == 15))
                                dst = (QT if kind == 0 else KT)[:, hd, o0:o0 + 512]
                                rms_T(T, C, C.ps_x[:], "ps_x", qg if kind == 0 else kg, dst, ("qk", kind, hd, o0), 128)
                        elif kind in (3, 4, 5):
                            for t in range(2):
                                for kc in range(16):
                                    T.op("pe", lambda e, kc=kc, hh=hh, t=t: e.matmul(
                                        C.ps_x[:], lhsT=wt[:, kc, hh * 128:(hh + 1) * 128], rhs=hT[:, kc, 1024 + 512 * t:1536 + 512 * t],
                                        start=(kc == 0), stop=(kc == 15)), reads=["wt"], writes=["ps_x"], sig=(kc == 15))
                                sl = uz[:, hd, 2 + 512 * t:2 + 512 * (t + 1)]
                                if kind == 3:
                                    T.op("act", lambda e, sl=sl: e.activation(out=sl, in_=C.ps_x[:], func=AF.Copy),
                                         reads=["ps_x"], writes=[("uz", hd, t)])
                                elif kind == 5:
                                    T.op("dve", lambda e, sl=sl: e.tensor_tensor(out=sl, in0=C.ps_x[:], in1=sl, op=ALU.mult),
                                         reads=["ps_x", ("uz", hd, t)], writes=[("uz", hd, t)])
                                else:
                                    pass
                            if kind in (3, 5):
                                for kc in range(16):
                                    T.op("pe", lambda e, kc=kc, hh=hh: e.matmul(
                                        C.ps_x[:, 0:2], lhsT=wt[:, kc, hh * 128:(hh + 1) * 128], rhs=hT[:, kc, 1022:1024],
                                        start=(kc == 0), stop=(kc == 15)), reads=["wt"], writes=["ps_x"], sig=(kc == 15))
                                sl = uz[:, hd, 0:2]
                                if kind == 3:
                                    T.op("act", lambda e, sl=sl: e.activation(out=sl, in_=C.ps_x[:, 0:2], func=AF.Copy),
                                         reads=["ps_x"], writes=[("uz", hd, "h")])
                                else:
                                    T.op("dve", lambda e, sl=sl: e.scalar_tensor_tensor(out=sl, in0=C.ps_x[:, 0:2], scalar=halo, in1=sl,
                                                                                      op0=ALU.mult, op1=ALU.mult),
                                         reads=["ps_x", ("uz", hd, "h")], writes=[("uz", hd, "h")])
                        else:
                            pass
                    if kind == 2:
                        for tt in range(16):
                            for kc in range(16):
                                T.op("pe", lambda e, kc=kc, tt=tt: e.matmul(
                                    C.ps_x[:], lhsT=hT[:, kc, tt * 128:(tt + 1) * 128], rhs=wt[:, kc, :],
                                    start=(kc == 0), stop=(kc == 15)), reads=["wt"], writes=["ps_x"], sig=(kc == 15))
                            T.op("act", lambda e, tt=tt, n=n: e.activation(out=V[:, tt, (n % 2) * 512:(n % 2 + 1) * 512], in_=C.ps_x[:], func=AF.Copy),
                                 reads=["ps_x"], writes=[("V", tt, n)])
                T.barrier()
                for n in (8, 9):
                    T.dma("pool", wt, _wview(w_in[:, n * 512:(n + 1) * 512]), writes=["wt"], dsem="wt")
                    for hh in range(4):
                        g = (n % 2) * 4 + hh
                        for t in range(2):
                            for kc in range(16):
                                T.op("pe", lambda e, kc=kc, hh=hh, t=t: e.matmul(
                                    C.ps_x[:], lhsT=wt[:, kc, hh * 128:(hh + 1) * 128], rhs=hT[:, kc, 1024 + 512 * t:1536 + 512 * t],
                                    start=(kc == 0), stop=(kc == 15)), reads=["wt"], writes=["ps_x"], sig=(kc == 15))
                            z0, z1, z2 = [uz[:, g, 2 + 512 * t - sh:2 + 512 * (t + 1) - sh] for sh in range(3)]
                            cz = zc[:, 0:512]
                            T.op("dve", lambda e, g=g, z0=z0: e.tensor_scalar(out=cz, in0=z0, scalar1=convw(g, 2), scalar2=None, op0=ALU.mult),
                                 reads=["uzall"], writes=["zc"])
                            T.op("dve", lambda e, g=g, z1=z1: e.scalar_tensor_tensor(out=cz, in0=z1, scalar=convw(g, 1), in1=cz, op0=ALU.mult, op1=ALU.add),
                                 reads=["zc"], writes=["zc"])
                            T.op("dve", lambda e, g=g, z2=z2: e.scalar_tensor_tensor(out=cz, in0=z2, scalar=convw(g, 0), in1=cz, op0=ALU.mult, op1=ALU.add),
                                 reads=["zc"], writes=["zc"])
                            T.op("dve", lambda e: e.tensor_tensor(out=cz, in0=C.ps_x[:], in1=cz, op=ALU.mult), reads=["ps_x", "zc"], writes=["zc"])
                            rms_T(T, C, cz, "zc", cog(g), yT[:, g, 512 * t:512 * (t + 1)], ("yT", g, t), 128)
                T.barrier()
            with ExitStack() as s3:
                sb3 = lambda n, s, d: sb(n, s, d, s3)
                kmf = sb3("kmf", [128, 64], F32)
                kmb = sb3("kmb", [128, 64], BF16)
                gbt = sb3("gbt", [128, 4 * 64], F32)
                es_f = sb3("es_f", [64, 2048], F32)
                es_b = sb3("es_b", [64, 64 * 128], BF16)
                cm_f = sb3("cm_f", [128, 2048], F32)
                cm_b = sb3("cm_b", [128, 2048], BF16)
                gb = sb3("gb", [128, 64], F32)
                mx = sb3("mx", [128, 64], F32)
                sel = sb3("sel", [128, 64], F32)
                val = sb3("val", [128, 64], F32)
                biasT = sb3("biasT", [64, NT], BF16)
                PT = [sb3("PT%d" % i, [128, 512], BF16) for i in range(3)]
                rden = sb3("rden", [128, 512], F32)
                a_f = sb3("a_f", [128, 512], F32)
                T.dma("sp", gbt[:], gbias, writes=["gbt"], dsem="c1a")
                for q4 in range(4):
                    T.dma("sp", es_f[:], esel[:, q4 * 2048:(q4 + 1) * 2048], writes=["es_f"], dsem="c1b")
                    T.op("act", lambda e, q4=q4: e.activation(out=es_b[:, q4 * 2048:(q4 + 1) * 2048], in_=es_f[:], func=AF.Copy),
                         reads=["es_f"], writes=[("es_b", q4)])
                T.dma("sp", cm_f[:], cmask, writes=["cm_f"], dsem="c1c")
                T.op("act", lambda e: e.activation(out=cm_b[:], in_=cm_f[:], func=AF.Copy), reads=["cm_f"], writes=["cm_b"])
                for hd in range(8):
                    T.op("dve", lambda e, hd=hd: e.tensor_reduce(out=kmf[:, hd * 8:(hd + 1) * 8],
                                                                 in_=KT[:, hd, :].rearrange("p (n k) -> p n k", k=256),
                                                                 axis=AX.X, op=ALU.add), writes=["kmf"])
                T.op("dve", lambda e: e.tensor_scalar(out=kmb[:], in0=kmf[:], scalar1=1.0 / 256, scalar2=None, op0=ALU.mult),
                     reads=["kmf"], writes=["kmb"])
                for qt in range(8):
                    j = 4 + qt // 2
                    for hd in range(8):
                        T.op("pe", lambda e, hd=hd, qt=qt: e.matmul(C.ps_x[:, hd * 8:(hd + 1) * 8], lhsT=QT[:, hd, qt * 128:(qt + 1) * 128],
                                                                    rhs=kmb[:, hd * 8:(hd + 1) * 8], start=True, stop=True),
                             reads=["kmb"], writes=["ps_x"], sig=(hd == 7))
                    T.op("dve", lambda e, qt=qt: e.tensor_tensor(out=gb[:], in0=C.ps_x[:, 0:64], in1=gbt[:, (qt // 2) * 64:(qt // 2 + 1) * 64], op=ALU.add),
                         reads=["ps_x", "gbt"], writes=["gb"])
                    for hd in range(8):
                        T.op("dve", lambda e, hd=hd: e.max(out=mx[:, hd * 8:(hd + 1) * 8], in_=gb[:, hd * 8:(hd + 1) * 8]),
                             reads=["gb"], writes=[("mx", hd)])
                        T.op("dve", lambda e, hd=hd: e.tensor_scalar(out=sel[:, hd * 8:(hd + 1) * 8], in0=gb[:, hd * 8:(hd + 1) * 8],
                                                                     scalar1=mx[:, hd * 8 + 2:hd * 8 + 3], scalar2=None, op0=ALU.is_ge),
                             reads=["gb", ("mx", hd)], writes=["sel"])
                    T.op("dve", lambda e: e.tensor_scalar(out=val[:], in0=gb[:], scalar1=-1e29, scalar2=None, op0=ALU.is_gt),
                         reads=["gb"], writes=["val"])
                    T.op("dve", lambda e: e.tensor_tensor(out=sel[:], in0=sel[:], in1=val[:], op=ALU.mult),
                         reads=["val", "sel"], writes=["sel"])
                    T.op("dve", lambda e, j=j: e.memset(sel[:].rearrange("p (h n) -> p h n", n=8)[:, :, j:j + 1], 1.0),
                         reads=["sel"], writes=["sel"])
                    T.op("dve", lambda e: e.tensor_scalar(out=sel[:], in0=sel[:], scalar1=-1.0, scalar2=-NEG, op0=ALU.add, op1=ALU.mult),
                         reads=["sel"], writes=["sel"])
                    T.op("pe", lambda e: e.transpose(out=C.ps_n[0:64, 0:128], in_=sel[:], identity=ident_f[:]),
                         reads=["sel"], writes=["ps_n"])
                    T.op("act", lambda e, qt=qt: e.activation(out=biasT[:, qt * 128:(qt + 1) * 128], in_=C.ps_n[0:64, 0:128], func=AF.Copy),
                         reads=["ps_n"], writes=[("biasT", qt)])
                T.barrier()
                pcnt = 0
                for hd in range(8):
                    for qc in range(2):
                        nk = 12 if qc == 0 else 16
                        for kt in range(nk):
                            r = kt - (8 + qc * 4)
                            diag = 0 <= r < 4
                            ps = C.ps_d[pcnt % 2]; pk = "ps_d%d" % (pcnt % 2)
                            pt = PT[pcnt % 3]; ptk = "PT%d" % (pcnt % 3)
                            pcnt += 1
                            T.op("pe", lambda e, ps=ps, hd=hd, kt=kt, qc=qc: e.matmul(
                                ps[:], lhsT=KT[:, hd, kt * 128:(kt + 1) * 128], rhs=QT[:, hd, qc * 512:(qc + 1) * 512], start=True, stop=False),
                                writes=[pk], sig=False)
                            T.op("pe", lambda e, ps=ps, hd=hd, kt=kt, qc=qc, diag=diag: e.matmul(
                                ps[:], lhsT=es_b[:, (hd * 8 + kt // 2) * 128:(hd * 8 + kt // 2 + 1) * 128], rhs=biasT[:, qc * 512:(qc + 1) * 512],
                                start=False, stop=(not diag)), reads=["es_b"], writes=[pk], sig=(not diag))
                            if diag:
                                T.op("pe", lambda e, ps=ps, r=r: e.matmul(ps[:], lhsT=C.ident_b[:], rhs=cm_b[:, r * 512:(r + 1) * 512],
                                                                          start=False, stop=True), reads=["cm_b"], writes=[pk])
                            T.op("act", lambda e, ps=ps, pt=pt: e.activation(out=pt[:], in_=ps[:], func=AF.Exp, scale=128.0 ** -0.5),
                                 reads=[pk], writes=[ptk])
                            T.op("pe", lambda e, pt=pt, hd=hd, kt=kt, nk=nk: e.matmul(
                                C.ps_g[:], lhsT=V[:, kt, hd * 128:(hd + 1) * 128], rhs=pt[:], start=(kt == 0), stop=(kt == nk - 1)),
                                reads=[ptk], writes=["ps_g"], sig=False)
                            T.op("pe", lambda e, pt=pt, kt=kt, nk=nk: e.matmul(
                                C.ps_u[:], lhsT=C.ones_b[:], rhs=pt[:], start=(kt == 0), stop=(kt == nk - 1)),
                                reads=[ptk], writes=["ps_u"])
                        T.op("dve", lambda e: e.reciprocal(out=rden[:], in_=C.ps_u[:]), reads=["ps_u"], writes=["rden"])
                        T.op("dve", lambda e: e.tensor_tensor(out=a_f[:], in0=C.ps_g[:], in1=rden[:], op=ALU.mult),
                             reads=["ps_g", "rden"], writes=["a_f"])
                        rms_T(T, C, a_f[:], "a_f", aog(hd), aT[:, hd, qc * 512:(qc + 1) * 512], ("aT", hd, qc), 128)
                T.barrier()
        if debug:
            o_a = nc.dram_tensor("o_a", [128, 8 * NT], BF16, kind="ExternalOutput").ap()
            o_y = nc.dram_tensor("o_y", [128, 8 * NT], BF16, kind="ExternalOutput").ap()
            T.dma("sp", o_a, aT[:].rearrange("p h t -> p (h t)"), writes=["o_a"], dsem="dbg0")
            T.dma("sp", o_y, yT[:].rearrange("p h t -> p (h t)"), writes=["o_y"], dsem="dbg1")
            T.barrier()
        x1 = sb("x1", [128, 8, D], F32)
        hfT = sb("hfT", [128, 16, NT], BF16)
        with ExitStack() as s4:
            g1bc = sb("g1bc", [128, D], F32, s4)
            wo = sb("wo", [128, 16, 512], BF16, s4)
            tmp = sb("tmp4", [128, 512], F32, s4)
            C.junk = sb("junk4", [128, 2048], BF16, s4)[:]
            C.xn = [sb("xn4_%d" % i, [128, 2048], BF16, s4) for i in range(2)]
            T.dma("sp", g1bc[:], g1row.partition_broadcast(128), writes=["g1bc"], dsem="c2")
            for tt in range(8):
                T.dma("sp", x1[:, tt, :], xo[tt * 128:(tt + 1) * 128, :], writes=[("x1", tt)], dsem="x1_%d" % tt)
            for n in range(4):
                T.dma("pool", wo[:], _wview(w_o[:, n * 512:(n + 1) * 512]), writes=["wo"], dsem="wt")
                for tt in range(8):
                    ps = C.ps_d[tt % 2]; pk = "ps_d%d" % (tt % 2)
                    for kc in range(16):
                        T.op("pe", lambda e, ps=ps, kc=kc, tt=tt: e.matmul(ps[:], lhsT=(aT if kc < 8 else yT)[:, kc % 8, tt * 128:(tt + 1) * 128], rhs=wo[:, kc, :],
                                                                         start=(kc == 0), stop=(kc == 15)),
                             reads=["wo"], writes=[pk], sig=(kc == 15))
                    T.op("dve", lambda e, ps=ps, n=n: e.tensor_tensor(out=tmp[:], in0=ps[:], in1=g1bc[:, n * 512:(n + 1) * 512], op=ALU.mult),
                         reads=[pk, "g1bc"], writes=["tmp4"])
                    T.op("dve", lambda e, tt=tt, n=n: e.tensor_tensor(out=x1[:, tt, n * 512:(n + 1) * 512], in0=x1[:, tt, n * 512:(n + 1) * 512],
                                                                     in1=tmp[:], op=ALU.add),
                         reads=["tmp4", ("x1", tt)], writes=[("x1", tt)])
            T.barrier()
            for tt in range(8):
                norm_transpose(T, C, x1[:, tt, :], ("x1", tt), 16 + tt, A2T, sh2T, hfT, tt * 128, "hfT")
            T.barrier()
        T.dma("sp", o_hfT.rearrange("(kc p) t -> p kc t", p=128), hfT[:], writes=["o_hfT"], dsem="o0")
        with ExitStack() as s5:
            sb5 = lambda n, s, d: sb(n, s, d, s5)
            wr = sb5("wr", [128, 16, 256], BF16)
            rb = sb5("rb", [128, 256], F32)
            sc = sb5("sc", [128, 256], F32)
            ch = sb5("ch", [128, 256], F32)
            mc = sb5("mc", [128, 256], F32)
            m8 = sb5("m8", [128, 64], F32)
            gs = sb5("gs", [128, 8], F32)
            gm = sb5("gm", [128, 8], F32)
            gk = sb5("gk", [128, 8], F32)
            t1 = sb5("t1", [128, 8], F32)
            t8 = sb5("t8", [128, 8], F32)
            den = sb5("den", [128, 1], F32)
            rw = sb5("rw", [128, 8, 256], F32)
            T.dma("pool", wr[:], _wview(w_r), writes=["wr"], dsem="wt")
            T.dma("sp", rb[:], rbias.partition_broadcast(128), writes=["rb"], dsem="c3")
            for tt in range(8):
                for kc in range(16):
                    T.op("pe", lambda e, kc=kc, tt=tt: e.matmul(C.ps_x[:, 0:256], lhsT=hfT[:, kc, tt * 128:(tt + 1) * 128], rhs=wr[:, kc, :],
                                                                start=(kc == 0), stop=(kc == 15)), reads=["wr"], writes=["ps_x"], sig=(kc == 15))
                T.op("act", lambda e: e.activation(out=sc[:], in_=C.ps_x[:, 0:256], func=AF.Sigmoid), reads=["ps_x"], writes=["sc"])
                T.op("dve", lambda e: e.tensor_tensor(out=ch[:], in0=sc[:], in1=rb[:], op=ALU.add), reads=["sc", "rb"], writes=["ch"])
                for g in range(8):
                    T.op("dve", lambda e, g=g: e.max(out=m8[:, g * 8:(g + 1) * 8], in_=ch[:, g * 32:(g + 1) * 32]), reads=["ch"], writes=[("m8", g)])
                m8v = m8[:].rearrange("p (g k) -> p g k", k=8)
                T.op("dve", lambda e: e.tensor_tensor(out=gs[:], in0=m8v[:, :, 0], in1=m8v[:, :, 1], op=ALU.add),
                     reads=[("m8", g) for g in range(8)], writes=["gs"])
                T.op("dve", lambda e: e.max(out=gm[:], in_=gs[:]), reads=["gs"], writes=["gm"])
                T.op("dve", lambda e: e.tensor_scalar(out=gk[:], in0=gs[:], scalar1=gm[:, 3:4], scalar2=None, op0=ALU.is_ge),
                     reads=["gs", "gm"], writes=["gk"])
                T.op("dve", lambda e: e.tensor_scalar(out=t1[:], in0=gk[:], scalar1=-1.0, scalar2=1e30, op0=ALU.add, op1=ALU.mult),
                     reads=["gk"], writes=["t1"])
                for g in range(8):
                    T.op("dve", lambda e, g=g: e.tensor_scalar(out=mc[:, g * 32:(g + 1) * 32], in0=ch[:, g * 32:(g + 1) * 32],
                                                               scalar1=gk[:, g:g + 1], scalar2=t1[:, g:g + 1], op0=ALU.mult, op1=ALU.add),
                         reads=["ch", "gk", "t1"], writes=["mc"])
                T.op("dve", lambda e: e.max(out=t8[:], in_=mc[:]), reads=["mc"], writes=["t8"])
                T.op("dve", lambda e: e.tensor_scalar(out=mc[:], in0=mc[:], scalar1=t8[:, 7:8], scalar2=None, op0=ALU.is_ge),
                     reads=["t8", "mc"], writes=["mc"])
                T.op("dve", lambda e: e.tensor_tensor(out=mc[:], in0=mc[:], in1=sc[:], op=ALU.mult), reads=["mc", "sc"], writes=["mc"])
                T.op("dve", lambda e: e.tensor_reduce(out=den[:], in_=mc[:], axis=AX.X, op=ALU.add), reads=["mc"], writes=["den"])
                T.op("dve", lambda e: e.reciprocal(out=den[:], in_=den[:]), reads=["den"], writes=["den"])
                T.op("dve", lambda e, tt=tt: e.tensor_scalar(out=rw[:, tt, :], in0=mc[:], scalar1=den[:, 0:1], scalar2=2.5, op0=ALU.mult, op1=ALU.mult),
                     reads=["mc", "den"], writes=[("rw", tt)])
            T.dma("sp", o_rw.rearrange("(tt p) e -> p tt e", p=128), rw[:], reads=[("rw", tt) for tt in range(8)], writes=["o_rw"], dsem="o1")
            T.barrier()
        with ExitStack() as s6:
            sb6 = lambda n, s, d: sb(n, s, d, s6)
            wg = sb6("wg", [128, 16, 512], BF16)
            wu = sb6("wu", [128, 16, 512], BF16)
            wd = sb6("wd", [128, 4, D], BF16)
            g2bc = sb6("g2bc", [128, D], F32)
            tmp = sb6("tmp6", [128, 512], F32)
            C.hid = sb6("hid", [128, 4, NT], BF16)
            T.dma("sp", g2bc[:], g2row.partition_broadcast(128), writes=["g2bc"], dsem="c4")
            T.dma("pool", wg[:], _wview(wsg), writes=["wg"], dsem="w6a")
            T.dma("pool", wu[:], _wview(wsu), writes=["wu"], dsem="w6b")
            T.dma("pool", wd[:], _wview(wsd), writes=["wd"], dsem="w6c")

            def epi(tt, n, ps, pk):
                T.op("dve", lambda e: e.tensor_tensor(out=tmp[:], in0=ps[:], in1=g2bc[:, n * 512:(n + 1) * 512], op=ALU.mult),
                     reads=[pk, "g2bc"], writes=["tmp6"])
                T.op("dve", lambda e: e.tensor_tensor(out=x1[:, tt, n * 512:(n + 1) * 512], in0=x1[:, tt, n * 512:(n + 1) * 512], in1=tmp[:], op=ALU.add),
                     reads=["tmp6"], writes=[("x1", tt)])

            expert_dense(T, C, hfT, "hfT", wg, "wg", wu, "wu", wd, "wd", epi)
            for tt in range(8):
                T.dma("sp", o_x1s[tt * 128:(tt + 1) * 128, :], x1[:, tt, :], reads=[("x1", tt)], writes=[("o_x1s", tt)], dsem="o2")
        T.emit()
    return nc


def build_fused(ne=256, debug=False):
    nc = bass.Bass("TRN2", target_bir_lowering=False)
    din = lambda n, s, d=F32: nc.dram_tensor(n, s, d, kind="ExternalInput").ap()
    xo = din("xo", [NT, D])
    xp = din("xp", [NT, D])
    cT = din("cT", [128, 16])
    w_ada = din("w_ada", [D, 6 * D])
    baT = din("baT", [128, 96])
    b_ada = din("b_ada", [1, 6 * D])
    nex = max(ne, 1)
    wg_d = din("wg", [nex * D, 512])
    wu_d = din("wu", [nex * D, 512])
    wd_d = din("wd", [nex * 512, D])
    gscr = nc.dram_tensor("gscr", [256, D], F32).ap()
    ngT = din("ngT", [128, 32])
    w_in = din("w_in", [D, 6144])
    smallT = din("smallT", [128, 64])
    w_o = din("w_o", [D, D])
    w_r = din("w_r", [D, 256])
    rbias = din("rbias", [1, 256])
    wsg = din("wsg", [D, 512])
    wsu = din("wsu", [D, 512])
    wsd = din("wsd", [512, D])
    ident = din("ident", [128, 128])
    gbias = din("gbias", [128, 4 * 64])
    esel = din("esel", [64, 64 * 128])
    cmask = din("cmask", [128, 4 * 512])
    y_out = nc.dram_tensor("y", [NT, D], F32, kind="ExternalOutput").ap()

    with ExitStack() as st:
        T = Tracker(nc, st)
        C = Ctx()
        sb = lambda n, s, d, stack=st: stack.enter_context(nc.sbuf_tensor(n, s, d))
        sbr = lambda n, s, d: st.enter_context(nc.sbuf_tensor(n, s, d, side="right"))
        psf = lambda n: st.enter_context(nc.psum_tensor(n, [128, 512], F32))
        C.ps_g = psf("ps_g"); C.ps_u = psf("ps_u"); C.ps_n = psf("ps_n")
        C.ps_d = [psf("ps_d0"), psf("ps_d1")]
        C.ps_x = psf("ps_x")
        C.psb = [st.enter_context(nc.psum_tensor("psb%d" % i, [128, 1024], BF16)) for i in range(2)]
        ident_f = sb("ident_f", [128, 128], F32)
        C.ident_b = sb("ident_b", [128, 128], BF16)
        C.ones_b = sb("ones_b", [128, 128], BF16)
        C.eps_t = sb("eps_t", [128, 1], F32)
        modv = sb("modv", [128, 96], F32)
        ng = sb("ng", [128, 32], F32)
        AT = sb("AT", [128, 32], F32)
        sm = sb("sm", [128, 64], F32)
        C.ss = sb("ss", [128, 32], F32)
        C.ss4 = sb("ss4", [128, 4], F32)
        C.sqb = sb("sqb", [128, 512], BF16)
        C.rs = sb("rs", [128, 512], F32)
        C.sg = sb("sg", [128, 512], F32)

        T.dma("sp", ident_f[:], ident, writes=["ident_f"], dsem="c0a")
        T.dma("sp", ng[:], ngT, writes=["ng"], dsem="c0c")
        T.dma("sp", sm[:], smallT, writes=["sm"], dsem="c0d")
        T.op("dve", lambda e: e.tensor_copy(out=C.ident_b[:], in_=ident_f[:]), reads=["ident_f"], writes=["ident_b"])
        T.op("pool", lambda e: e.memset(C.ones_b[:], 1.0), writes=["ones_b"])
        T.op("pool", lambda e: e.memset(C.eps_t[:], EPS), writes=["eps_t"])
        with ExitStack() as s0:
            sb0 = lambda n, s, d: sb(n, s, d, s0)
            c_f = sb0("c_f", [128, 16], F32)
            c_s = sb0("c_s", [128, 16], F32)
            c_b = sb0("c_b", [128, 16], BF16)
            crep = sb0("crep", [128, 16, 128], BF16)
            bat = sb0("bat", [128, 96], F32)
            bbc = sb0("bbc", [128, 512], F32)
            gt = sb0("gt", [128, 512], F32)
            was = [sb0("wa%d" % i, [128, 16, 512], BF16) for i in range(2)]
            T.dma("sp", c_f[:], cT, writes=["c_f"], dsem="a0")
            T.dma("sp", bat[:], baT, writes=["bat"], dsem="a1")
            T.op("act", lambda e: e.activation(out=c_s[:], in_=c_f[:], func=AF.Silu), reads=["c_f"], writes=["c_s"])
            T.op("dve", lambda e: e.tensor_copy(out=c_b[:], in_=c_s[:]), reads=["c_s"], writes=["c_b"])
            for kc in range(16):
                T.op("dve", lambda e, kc=kc: e.tensor_scalar(out=crep[:, kc, :], in0=C.ones_b[:], scalar1=c_s[:, kc:kc + 1], scalar2=None, op0=ALU.mult),
                     reads=["c_s", "ones_b"], writes=["crep"])
            for n in range(24):
                wa = was[n % 2]
                wk = "wa%d" % (n % 2)
                T.dma("pool", wa[:], _wview(w_ada[:, n * 512:(n + 1) * 512]), writes=[wk], dsem=wk)
                if (n // 4) in (2, 5):
                    gi = 0 if n // 4 == 2 else 1
                    T.dma("sp", bbc[:], b_ada[0:1, n * 512:(n + 1) * 512].partition_broadcast(128), writes=["bbc"], dsem="a2")
                    for kc in range(16):
                        T.op("pe", lambda e, kc=kc, wa=wa: e.matmul(C.ps_g[:], lhsT=crep[:, kc, :], rhs=wa[:, kc, :], start=(kc == 0), stop=(kc == 15)),
                             reads=[wk, "crep"], writes=["ps_g"], sig=(kc == 15))
                    T.op("dve", lambda e: e.tensor_tensor(out=gt[:], in0=C.ps_g[:], in1=bbc[:], op=ALU.add), reads=["ps_g", "bbc"], writes=["gt"])
                    T.dma("sp", gscr[gi * 128:(gi + 1) * 128, (n % 4) * 512:(n % 4 + 1) * 512], gt[:], reads=["gt"], writes=[("gscr", n)], dsem="a3")
                else:
                    for j in range(4):
                        for kc in range(16):
                            T.op("pe", lambda e, kc=kc, j=j, wa=wa: e.matmul(C.ps_x[:, j:j + 1], lhsT=wa[:, kc, j * 128:(j + 1) * 128], rhs=c_b[:, kc:kc + 1],
                                                                           start=(kc == 0), stop=(kc == 15)),
                                 reads=[wk, "c_b"], writes=["ps_x"], sig=(kc == 15))
                    T.op("dve", lambda e, n=n: e.tensor_tensor(out=modv[:, n * 4:n * 4 + 4], in0=C.ps_x[:, 0:4], in1=bat[:, n * 4:n * 4 + 4], op=ALU.add),
                         reads=["ps_x", "bat"], writes=["modv0"])
            T.barrier()
        T.op("dve", lambda e: e.tensor_scalar(out=AT[:, 0:16], in0=modv[:, 16:32], scalar1=1.0, scalar2=None, op0=ALU.add),
             reads=["modv0"], writes=["AT"])
        T.op("dve", lambda e: e.tensor_scalar(out=AT[:, 16:32], in0=modv[:, 64:80], scalar1=1.0, scalar2=None, op0=ALU.add),
             reads=["modv0"], writes=["AT"])
        T.op("dve", lambda e: e.tensor_tensor(out=AT[:], in0=AT[:], in1=ng[:], op=ALU.mult), reads=["AT", "ng"], writes=["AT"])
        T.barrier()
        A1T = AT[:, 0:16]; A2T = AT[:, 16:32]
        sh1T = modv[:, 0:16]; sh2T = modv[:, 48:64]
        qg = sm[:, 0:1]; kg = sm[:, 1:2]
        convw = lambda g, j: sm[:, 2 + g * 3 + j:3 + g * 3 + j]
        aog = lambda h: sm[:, 26 + h:27 + h]
        cog = lambda g: sm[:, 34 + g:35 + g]
        halo = sm[:, 42:43]

        yT = sb("yT", [128, 8, NT], BF16)
        aT = sb("aT", [128, 8, NT], BF16)
        with ExitStack() as sA:
            sbA = lambda n, s, d: sb(n, s, d, sA)
            QT = sbA("QT", [128, 8, NT], BF16)
            KT = sbA("KT", [128, 8, 2048], BF16)
            V = sbA("V", [128, 16, 1024], BF16)
            with ExitStack() as sP:
                sbP = lambda n, s, d: sb(n, s, d, sP)
                hT = sbP("hT", [128, 16, 2048], BF16)
                with ExitStack() as sX:
                    xb = [sb("xb%d" % i, [128, 2048], F32, sX) for i in range(2)]
                    C.junk = aT[:, 0:2, :].rearrange("p h t -> p (h t)")
                    _xn = sb("xn0", [128, 2048], BF16, sX)
                    C.xn = [_xn, _xn]
                    for i in range(16):
                        src = xp if i < 8 else xo
                        j = i % 8
                        T.dma("sp", xb[i % 2][:], src[j * 128:(j + 1) * 128, :], writes=["xb%d" % (i % 2)], dsem="x%d" % (i % 2))
                        norm_transpose(T, C, xb[i % 2][:], "xb%d" % (i % 2), i, A1T, sh1T, hT, i * 128, "hT")
                    T.barrier()
                wt = aT[:].rearrange("p h t -> p (h t)").rearrange("p (kc n) -> p kc n", n=512)
                if debug:
                    o_h = nc.dram_tensor("o_h", [128, 16 * 2048], BF16, kind="ExternalOutput").ap()
                    T.dma("sp", o_h, hT[:].rearrange("p c t -> p (c t)"), writes=["o_h"], dsem="dbg2")
                    T.barrier()
                uz = sbP("uz", [128, 8, NT + 2], BF16)
                zc = sbP("zc", [128, 512], F32)
                for n in (0, 1, 2, 3, 4, 5, 6, 7, 10, 11):
                    T.dma("pool", wt, _wview(w_in[:, n * 512:(n + 1) * 512]), writes=["wt"], dsem="wt")
                    kind = n // 2
                    for hh in range(4):
                        hd = (n % 2) * 4 + hh
                        if kind in (0, 1):
                            chunks = [(1024 + 512 * t, 512 * t) for t in range(2)] if kind == 0 else [(512 * t, 512 * t) for t in range(4)]
                            for (c0, o0) in chunks:
                                for kc in range(16):
                                    T.op("pe", lambda e, kc=kc, hh=hh, c0=c0: e.matmul(
                                        C.ps_x[:], lhsT=wt[:, kc, hh * 128:(hh + 1) * 128], rhs=hT[:, kc, c0:c0 + 512],
                                        start=(kc == 0), stop=(kc == 15)), reads=["wt"], writes=["ps_x"], sig=(kc == 15))
                                dst = (QT if kind == 0 else KT)[:, hd, o0:o0 + 512]
                                rms_T(T, C, C.ps_x[:], "ps_x", qg if kind == 0 else kg, dst, ("qk", kind, hd, o0), 128)
                        elif kind in (3, 4, 5):
                            for t in range(2):
                                for kc in range(16):
                                    T.op("pe", lambda e, kc=kc, hh=hh, t=t: e.matmul(
                                        C.ps_x[:], lhsT=wt[:, kc, hh * 128:(hh + 1) * 128], rhs=hT[:, kc, 1024 + 512 * t:1536 + 512 * t],
                                        start=(kc == 0), stop=(kc == 15)), reads=["wt"], writes=["ps_x"], sig=(kc == 15))
                                sl = uz[:, hd, 2 + 512 * t:2 + 512 * (t + 1)]
                                if kind == 3:
                                    T.op("act", lambda e, sl=sl: e.activation(out=sl, in_=C.ps_x[:], func=AF.Copy),
                                         reads=["ps_x"], writes=[("uz", hd, t)])
                                elif kind == 5:
                                    T.op("dve", lambda e, sl=sl: e.tensor_tensor(out=sl, in0=C.ps_x[:], in1=sl, op=ALU.mult),
                                         reads=["ps_x", ("uz", hd, t)], writes=[("uz", hd, t)])
                                else:
                                    pass
                            if kind in (3, 5):
                                for kc in range(16):
                                    T.op("pe", lambda e, kc=kc, hh=hh: e.matmul(
                                        C.ps_x[:, 0:2], lhsT=wt[:, kc, hh * 128:(hh + 1) * 128], rhs=hT[:, kc, 1022:1024],
                                        start=(kc == 0), stop=(kc == 15)), reads=["wt"], writes=["ps_x"], sig=(kc == 15))
                                sl = uz[:, hd, 0:2]
                                if kind == 3:
                                    T.op("act", lambda e, sl=sl: e.activation(out=sl, in_=C.ps_x[:, 0:2], func=AF.Copy),
                                         reads=["ps_x"], writes=[("uz", hd, "h")])
                                else:
                                    T.op("dve", lambda e, sl=sl: e.scalar_tensor_tensor(out=sl, in0=C.ps_x[:, 0:2], scalar=halo, in1=sl,
                                                                                      op0=ALU.mult, op1=ALU.mult),
                                         reads=["ps_x", ("uz", hd, "h")], writes=[("uz", hd, "h")])
                        else:
                            pass
                    if kind == 2:
                        for tt in range(16):
                            for kc in range(16):
                                T.op("pe", lambda e, kc=kc, tt=tt: e.matmul(
                                    C.ps_x[:], lhsT=hT[:, kc, tt * 128:(tt + 1) * 128], rhs=wt[:, kc, :],
                                    start=(kc == 0), stop=(kc == 15)), reads=["wt"], writes=["ps_x"], sig=(kc == 15))
                            T.op("act", lambda e, tt=tt, n=n: e.activation(out=V[:, tt, (n % 2) * 512:(n % 2 + 1) * 512], in_=C.ps_x[:], func=AF.Copy),
                                 reads=["ps_x"], writes=[("V", tt, n)])
                T.barrier()
                for n in (8, 9):
                    T.dma("pool", wt, _wview(w_in[:, n * 512:(n + 1) * 512]), writes=["wt"], dsem="wt")
                    for hh in range(4):
                        g = (n % 2) * 4 + hh
                        for t in range(2):
                            for kc in range(16):
                                T.op("pe", lambda e, kc=kc, hh=hh, t=t: e.matmul(
                                    C.ps_x[:], lhsT=wt[:, kc, hh * 128:(hh + 1) * 128], rhs=hT[:, kc, 1024 + 512 * t:1536 + 512 * t],
                                    start=(kc == 0), stop=(kc == 15)), reads=["wt"], writes=["ps_x"], sig=(kc == 15))
                            z0, z1, z2 = [uz[:, g, 2 + 512 * t - sh:2 + 512 * (t + 1) - sh] for sh in range(3)]
                            cz = zc[:, 0:512]
                            T.op("dve", lambda e, g=g, z0=z0: e.tensor_scalar(out=cz, in0=z0, scalar1=convw(g, 2), scalar2=None, op0=ALU.mult),
                                 reads=["uzall"], writes=["zc"])
                            T.op("dve", lambda e, g=g, z1=z1: e.scalar_tensor_tensor(out=cz, in0=z1, scalar=convw(g, 1), in1=cz, op0=ALU.mult, op1=ALU.add),
                                 reads=["zc"], writes=["zc"])
                            T.op("dve", lambda e, g=g, z2=z2: e.scalar_tensor_tensor(out=cz, in0=z2, scalar=convw(g, 0), in1=cz, op0=ALU.mult, op1=ALU.add),
                                 reads=["zc"], writes=["zc"])
                            T.op("dve", lambda e: e.tensor_tensor(out=cz, in0=C.ps_x[:], in1=cz, op=ALU.mult), reads=["ps_x", "zc"], writes=["zc"])
                            rms_T(T, C, cz, "zc", cog(g), yT[:, g, 512 * t:512 * (t + 1)], ("yT", g, t), 128)
                T.barrier()
            with ExitStack() as s3:
                sb3 = lambda n, s, d: sb(n, s, d, s3)
                kmf = sb3("kmf", [128, 64], F32)
                kmb = sb3("kmb", [128, 64], BF16)
                gbt = sb3("gbt", [128, 4 * 64], F32)
                es_f = sb3("es_f", [64, 2048], F32)
                es_b = sb3("es_b", [64, 64 * 128], BF16)
                cm_f = sb3("cm_f", [128, 2048], F32)
                cm_b = sb3("cm_b", [128, 2048], BF16)
                gb = sb3("gb", [128, 64], F32)
                mx = sb3("mx", [128, 64], F32)
                sel = sb3("sel", [128, 64], F32)
                val = sb3("val", [128, 64], F32)
                biasT = sb3("biasT", [64, NT], BF16)
                PT = [sb3("PT%d" % i, [128, 512], BF16) for i in range(3)]
                rden = sb3("rden", [128, 512], F32)
                a_f = sb3("a_f", [128, 512], F32)
                T.dma("sp", gbt[:], gbias, writes=["gbt"], dsem="c1a")
                for q4 in range(4):
                    T.dma("sp", es_f[:], esel[:, q4 * 2048:(q4 + 1) * 2048], writes=["es_f"], dsem="c1b")
                    T.op("act", lambda e, q4=q4: e.activation(out=es_b[:, q4 * 2048:(q4 + 1) * 2048], in_=es_f[:], func=AF.Copy),
                         reads=["es_f"], writes=[("es_b", q4)])
                T.dma("sp", cm_f[:], cmask, writes=["cm_f"], dsem="c1c")
                T.op("act", lambda e: e.activation(out=cm_b[:], in_=cm_f[:], func=AF.Copy), reads=["cm_f"], writes=["cm_b"])
                for hd in range(8):
                    T.op("dve", lambda e, hd=hd: e.tensor_reduce(out=kmf[:, hd * 8:(hd + 1) * 8],
                                                                 in_=KT[:, hd, :].rearrange("p (n k) -> p n k", k=256),
                                                                 axis=AX.X, op=ALU.add), writes=["kmf"])
                T.op("dve", lambda e: e.tensor_scalar(out=kmb[:], in0=kmf[:], scalar1=1.0 / 256, scalar2=None, op0=ALU.mult),
                     reads=["kmf"], writes=["kmb"])
                for qt in range(8):
                    j = 4 + qt // 2
                    for hd in range(8):
                        T.op("pe", lambda e, hd=hd, qt=qt: e.matmul(C.ps_x[:, hd * 8:(hd + 1) * 8], lhsT=QT[:, hd, qt * 128:(qt + 1) * 128],
                                                                    rhs=kmb[:, hd * 8:(hd + 1) * 8], start=True, stop=True),
                             reads=["kmb"], writes=["ps_x"], sig=(hd == 7))
                    T.op("dve", lambda e, qt=qt: e.tensor_tensor(out=gb[:], in0=C.ps_x[:, 0:64], in1=gbt[:, (qt // 2) * 64:(qt // 2 + 1) * 64], op=ALU.add),
                         reads=["ps_x", "gbt"], writes=["gb"])
                    for hd in range(8):
                        T.op("dve", lambda e, hd=hd: e.max(out=mx[:, hd * 8:(hd + 1) * 8], in_=gb[:, hd * 8:(hd + 1) * 8]),
                             reads=["gb"], writes=[("mx", hd)])
                        T.op("dve", lambda e, hd=hd: e.tensor_scalar(out=sel[:, hd * 8:(hd + 1) * 8], in0=gb[:, hd * 8:(hd + 1) * 8],
                                                                     scalar1=mx[:, hd * 8 + 2:hd * 8 + 3], scalar2=None, op0=ALU.is_ge),
                             reads=["gb", ("mx", hd)], writes=["sel"])
                    T.op("dve", lambda e: e.tensor_scalar(out=val[:], in0=gb[:], scalar1=-1e29, scalar2=None, op0=ALU.is_gt),
                         reads=["gb"], writes=["val"])
                    T.op("dve", lambda e: e.tensor_tensor(out=sel[:], in0=sel[:], in1=val[:], op=ALU.mult),
                         reads=["val", "sel"], writes=["sel"])
                    T.op("dve", lambda e, j=j: e.memset(sel[:].rearrange("p (h n) -> p h n", n=8)[:, :, j:j + 1], 1.0),
                         reads=["sel"], writes=["sel"])
                    T.op("dve", lambda e: e.tensor_scalar(out=sel[:], in0=sel[:], scalar1=-1.0, scalar2=-NEG, op0=ALU.add, op1=ALU.mult),
                         reads=["sel"], writes=["sel"])
                    T.op("pe", lambda e: e.transpose(out=C.ps_n[0:64, 0:128], in_=sel[:], identity=ident_f[:]),
                         reads=["sel"], writes=["ps_n"])
                    T.op("act", lambda e, qt=qt: e.activation(out=biasT[:, qt * 128:(qt + 1) * 128], in_=C.ps_n[0:64, 0:128], func=AF.Copy),
                         reads=["ps_n"], writes=[("biasT", qt)])
                T.barrier()
                pcnt = 0
                for hd in range(8):
                    for qc in range(2):
                        nk = 12 if qc == 0 else 16
                        for kt in range(nk):
                            r = kt - (8 + qc * 4)
                            diag = 0 <= r < 4
                            ps = C.ps_d[pcnt % 2]; pk = "ps_d%d" % (pcnt % 2)
                            pt = PT[pcnt % 3]; ptk = "PT%d" % (pcnt % 3)
                            pcnt += 1
                            T.op("pe", lambda e, ps=ps, hd=hd, kt=kt, qc=qc: e.matmul(
                                ps[:], lhsT=KT[:, hd, kt * 128:(kt + 1) * 128], rhs=QT[:, hd, qc * 512:(qc + 1) * 512], start=True, stop=False),
                                writes=[pk], sig=False)
                            T.op("pe", lambda e, ps=ps, hd=hd, kt=kt, qc=qc, diag=diag: e.matmul(
                                ps[:], lhsT=es_b[:, (hd * 8 + kt // 2) * 128:(hd * 8 + kt // 2 + 1) * 128], rhs=biasT[:, qc * 512:(qc + 1) * 512],
                                start=False, stop=(not diag)), reads=["es_b"], writes=[pk], sig=(not diag))
                            if diag:
                                T.op("pe", lambda e, ps=ps, r=r: e.matmul(ps[:], lhsT=C.ident_b[:], rhs=cm_b[:, r * 512:(r + 1) * 512],
                                                                          start=False, stop=True), reads=["cm_b"], writes=[pk])
                            T.op("act", lambda e, ps=ps, pt=pt: e.activation(out=pt[:], in_=ps[:], func=AF.Exp, scale=128.0 ** -0.5),
                                 reads=[pk], writes=[ptk])
                            T.op("pe", lambda e, pt=pt, hd=hd, kt=kt, nk=nk: e.matmul(
                                C.ps_g[:], lhsT=V[:, kt, hd * 128:(hd + 1) * 128], rhs=pt[:], start=(kt == 0), stop=(kt == nk - 1)),
                                reads=[ptk], writes=["ps_g"], sig=False)
                            T.op("pe", lambda e, pt=pt, kt=kt, nk=nk: e.matmul(
                                C.ps_u[:], lhsT=C.ones_b[:], rhs=pt[:], start=(kt == 0), stop=(kt == nk - 1)),
                                reads=[ptk], writes=["ps_u"])
                        T.op("dve", lambda e: e.reciprocal(out=rden[:], in_=C.ps_u[:]), reads=["ps_u"], writes=["rden"])
                        T.op("dve", lambda e: e.tensor_tensor(out=a_f[:], in0=C.ps_g[:], in1=rden[:], op=ALU.mult),
                             reads=["ps_g", "rden"], writes=["a_f"])
                        rms_T(T, C, a_f[:], "a_f", aog(hd), aT[:, hd, qc * 512:(qc + 1) * 512], ("aT", hd, qc), 128)
                T.barrier()
        if debug:
            o_a = nc.dram_tensor("o_a", [128, 8 * NT], BF16, kind="ExternalOutput").ap()
            o_y = nc.dram_tensor("o_y", [128, 8 * NT], BF16, kind="ExternalOutput").ap()
            T.dma("sp", o_a, aT[:].rearrange("p h t -> p (h t)"), writes=["o_a"], dsem="dbg0")
            T.dma("sp", o_y, yT[:].rearrange("p h t -> p (h t)"), writes=["o_y"], dsem="dbg1")
            T.barrier()
        x1 = sbr("x1", [128, 8, D], F32)
        hfT = sbr("hfT", [128, 16, NT], BF16)
        rw = sbr("rw", [128, 8, 256], F32)
        g2bc = sbr("g2bc", [128, D], F32)
        with ExitStack() as s4:
            g1bc = sb("g1bc", [128, D], F32, s4)
            wo = sb("wo", [128, 16, 512], BF16, s4)
            tmp = sb("tmp4", [128, 512], F32, s4)
            C.junk = sb("junk4", [128, 2048], BF16, s4)[:]
            C.xn = [sb("xn4_%d" % i, [128, 2048], BF16, s4) for i in range(2)]
            T.dma("sp", g1bc[:], gscr[0:128, :], writes=["g1bc"], dsem="c2")
            for tt in range(8):
                T.dma("sp", x1[:, tt, :], xo[tt * 128:(tt + 1) * 128, :], writes=[("x1", tt)], dsem="x1_%d" % tt)
            for n in range(4):
                T.dma("pool", wo[:], _wview(w_o[:, n * 512:(n + 1) * 512]), writes=["wo"], dsem="wt")
                for tt in range(8):
                    ps = C.ps_d[tt % 2]; pk = "ps_d%d" % (tt % 2)
                    for kc in range(16):
                        T.op("pe", lambda e, ps=ps, kc=kc, tt=tt: e.matmul(ps[:], lhsT=(aT if kc < 8 else yT)[:, kc % 8, tt * 128:(tt + 1) * 128], rhs=wo[:, kc, :],
                                                                         start=(kc == 0), stop=(kc == 15)),
                             reads=["wo"], writes=[pk], sig=(kc == 15))
                    T.op("dve", lambda e, ps=ps, n=n: e.tensor_tensor(out=tmp[:], in0=ps[:], in1=g1bc[:, n * 512:(n + 1) * 512], op=ALU.mult),
                         reads=[pk, "g1bc"], writes=["tmp4"])
                    T.op("dve", lambda e, tt=tt, n=n: e.tensor_tensor(out=x1[:, tt, n * 512:(n + 1) * 512], in0=x1[:, tt, n * 512:(n + 1) * 512],
                                                                     in1=tmp[:], op=ALU.add),
                         reads=["tmp4", ("x1", tt)], writes=[("x1", tt)])
            T.barrier()
            for tt in range(8):
                norm_transpose(T, C, x1[:, tt, :], ("x1", tt), 16 + tt, A2T, sh2T, hfT, tt * 128, "hfT")
            T.barrier()
        with ExitStack() as s5:
            sb5 = lambda n, s, d: sb(n, s, d, s5)
            wr = sb5("wr", [128, 16, 256], BF16)
            rb = sb5("rb", [128, 256], F32)
            sc = sb5("sc", [128, 256], F32)
            ch = sb5("ch", [128, 256], F32)
            mc = sb5("mc", [128, 256], F32)
            m8 = sb5("m8", [128, 64], F32)
            gs = sb5("gs", [128, 8], F32)
            gm = sb5("gm", [128, 8], F32)
            gk = sb5("gk", [128, 8], F32)
            t1 = sb5("t1", [128, 8], F32)
            t8 = sb5("t8", [128, 8], F32)
            den = sb5("den", [128, 1], F32)
            T.dma("pool", wr[:], _wview(w_r), writes=["wr"], dsem="wt")
            T.dma("sp", rb[:], rbias.partition_broadcast(128), writes=["rb"], dsem="c3")
            for tt in range(8):
                for kc in range(16):
                    T.op("pe", lambda e, kc=kc, tt=tt: e.matmul(C.ps_x[:, 0:256], lhsT=hfT[:, kc, tt * 128:(tt + 1) * 128], rhs=wr[:, kc, :],
                                                                start=(kc == 0), stop=(kc == 15)), reads=["wr"], writes=["ps_x"], sig=(kc == 15))
                T.op("act", lambda e: e.activation(out=sc[:], in_=C.ps_x[:, 0:256], func=AF.Sigmoid), reads=["ps_x"], writes=["sc"])
                T.op("dve", lambda e: e.tensor_tensor(out=ch[:], in0=sc[:], in1=rb[:], op=ALU.add), reads=["sc", "rb"], writes=["ch"])
                for g in range(8):
                    T.op("dve", lambda e, g=g: e.max(out=m8[:, g * 8:(g + 1) * 8], in_=ch[:, g * 32:(g + 1) * 32]), reads=["ch"], writes=[("m8", g)])
                m8v = m8[:].rearrange("p (g k) -> p g k", k=8)
                T.op("dve", lambda e: e.tensor_tensor(out=gs[:], in0=m8v[:, :, 0], in1=m8v[:, :, 1], op=ALU.add),
                     reads=[("m8", g) for g in range(8)], writes=["gs"])
                T.op("dve", lambda e: e.max(out=gm[:], in_=gs[:]), reads=["gs"], writes=["gm"])
                T.op("dve", lambda e: e.tensor_scalar(out=gk[:], in0=gs[:], scalar1=gm[:, 3:4], scalar2=None, op0=ALU.is_ge),
                     reads=["gs", "gm"], writes=["gk"])
                T.op("dve", lambda e: e.tensor_scalar(out=t1[:], in0=gk[:], scalar1=-1.0, scalar2=1e30, op0=ALU.add, op1=ALU.mult),
                     reads=["gk"], writes=["t1"])
                for g in range(8):
                    T.op("dve", lambda e, g=g: e.tensor_scalar(out=mc[:, g * 32:(g + 1) * 32], in0=ch[:, g * 32:(g + 1) * 32],
                                                               scalar1=gk[:, g:g + 1], scalar2=t1[:, g:g + 1], op0=ALU.mult, op1=ALU.add),
                         reads=["ch", "gk", "t1"], writes=["mc"])
                T.op("dve", lambda e: e.max(out=t8[:], in_=mc[:]), reads=["mc"], writes=["t8"])
                T.op("dve", lambda e: e.tensor_scalar(out=mc[:], in0=mc[:], scalar1=t8[:, 7:8], scalar2=None, op0=ALU.is_ge),
                     reads=["t8", "mc"], writes=["mc"])
                T.op("dve", lambda e: e.tensor_tensor(out=mc[:], in0=mc[:], in1=sc[:], op=ALU.mult), reads=["mc", "sc"], writes=["mc"])
                T.op("dve", lambda e: e.tensor_reduce(out=den[:], in_=mc[:], axis=AX.X, op=ALU.add), reads=["mc"], writes=["den"])
                T.op("dve", lambda e: e.reciprocal(out=den[:], in_=den[:]), reads=["den"], writes=["den"])
                T.op("dve", lambda e, tt=tt: e.tensor_scalar(out=rw[:, tt, :], in0=mc[:], scalar1=den[:, 0:1], scalar2=2.5, op0=ALU.mult, op1=ALU.mult),
                     reads=["mc", "den"], writes=[("rw", tt)])
            T.barrier()
        with ExitStack() as s6:
            sb6 = lambda n, s, d: sb(n, s, d, s6)
            rA = sb6("ringA", [128, 8192], BF16)
            rB = sb6("ringB", [128, 8192], BF16)
            ring = [aT[:].rearrange("p h t -> p (h t)"), yT[:].rearrange("p h t -> p (h t)"), rA[:], rB[:]]
            onec = sb6("onec", [128, 1], F32)
            C.hid = sb6("hid", [128, 4, NT], BF16)
            T.op("pool", lambda e: e.memset(onec[:], 1.0), writes=["onec"])
            T.dma("sp", g2bc[:], gscr[128:256, :], writes=["g2bc"], dsem="c4")
            rcnt = [0]
            pbanks = [(C.ps_d[0], "ps_d0"), (C.ps_d[1], "ps_d1"), (C.ps_n, "ps_n"), (C.ps_x, "ps_x")]
            pcnt6 = [0]

            def load_w(src2d, nk):
                i = rcnt[0] % 4
                rcnt[0] += 1
                v = ring[i].rearrange("p (kc n) -> p kc n", n=(512 if nk == 16 else D))
                T.dma("pool", v, _wview(src2d), writes=["ring%d" % i], dsem="ring%d" % i)
                return v, "ring%d" % i

            for ex in list(range(ne)) + [256]:
                if ex < 256:
                    wg, wgk = load_w(wg_d[ex * D:(ex + 1) * D, :], 16)
                    wu, wuk = load_w(wu_d[ex * D:(ex + 1) * D, :], 16)
                    wd, wdk = load_w(wd_d[ex * 512:(ex + 1) * 512, :], 4)
                    scal_of_tt = lambda tt, ex=ex: rw[:, tt, ex:ex + 1]
                    scal_key = ("rw", "all")
                else:
                    wg, wgk = load_w(wsg, 16)
                    wu, wuk = load_w(wsu, 16)
                    wd, wdk = load_w(wsd, 4)
                    scal_of_tt = lambda tt: onec[:, 0:1]
                    scal_key = "onec"
                expert_dense2(T, C, hfT, "hfT", wg, wgk, wu, wuk, wd, wdk, g2bc, scal_of_tt, scal_key, x1, pbanks, pcnt6)
            for tt in range(8):
                T.dma("sp", y_out[tt * 128:(tt + 1) * 128, :], x1[:, tt, :], reads=[("x1", tt, n) for n in range(4)],
                      writes=[("y", tt)], dsem="o2")
        T.emit()
    return nc


def build_l2():
    nc = bass.Bass("TRN2", target_bir_lowering=False)
    hfa = nc.dram_tensor("hfa", [NCORES * D, NT], BF16, kind="ExternalInput").ap()
    rwl = nc.dram_tensor("rwl", [NCORES * NT, 32], F32, kind="ExternalInput").ap()
    wg_d = nc.dram_tensor("wg", [32 * D, 512], F32, kind="ExternalInput").ap()
    wu_d = nc.dram_tensor("wu", [32 * D, 512], F32, kind="ExternalInput").ap()
    wd_d = nc.dram_tensor("wd", [32 * 512, D], F32, kind="ExternalInput").ap()
    part = nc.dram_tensor("part", [NCORES * NT, D], F32, kind="ExternalOutput").ap()
    with ExitStack() as st:
        T = Tracker(nc, st)
        C = Ctx()
        sb = lambda n, s, d: st.enter_context(nc.sbuf_tensor(n, s, d))
        psf = lambda n: st.enter_context(nc.psum_tensor(n, [128, 512], F32))
        C.ps_g = psf("ps_g"); C.ps_u = psf("ps_u")
        C.ps_d = [psf("ps_d0"), psf("ps_d1")]
        C.sg = sb("sg", [128, 512], F32)
        C.hid = sb("hid", [128, 4, NT], BF16)
        hT = sb("hT", [128, 16, NT], BF16)
        acc = sb("acc", [128, 8, D], F32)
        rw = sb("rw", [128, 8, 32], F32)
        ring = [sb("ring%d" % i, [128, 8192], BF16) for i in range(4)]
        rcnt = [0]

        def load_w(src2d, shape3):
            i = rcnt[0] % 4
            rcnt[0] += 1
            t = ring[i]
            if shape3 == 16:
                v = t[:].rearrange("p (kc n) -> p kc n", n=512)
            else:
                v = t[:].rearrange("p (kc n) -> p kc n", n=D)
            T.dma("pool", v, _wview(src2d), writes=["ring%d" % i], dsem="ring%d" % i)
            return v, "ring%d" % i

        for r in range(NCORES):
            T.dma("sp", hT[:], hfa[r * D:(r + 1) * D, :].rearrange("(kc p) t -> p kc t", p=128), writes=["hT"], dsem="h")
            T.dma("sp", rw[:], rwl[r * NT:(r + 1) * NT, :].rearrange("(tt p) e -> p tt e", p=128), writes=["rw"], dsem="hr")
            for ex in range(32):
                wg, wgk = load_w(wg_d[ex * D:(ex + 1) * D, :], 16)
                wu, wuk = load_w(wu_d[ex * D:(ex + 1) * D, :], 16)
                wd, wdk = load_w(wd_d[ex * 512:(ex + 1) * 512, :], 4)

                def epi(tt, n, ps, pk, ex=ex):
                    a = acc[:, tt, n * 512:(n + 1) * 512]
                    if ex == 0:
                        T.op("dve", lambda e: e.tensor_scalar(out=a, in0=ps[:], scalar1=rw[:, tt, ex:ex + 1], scalar2=None, op0=ALU.mult),
                             reads=[pk, "rw"], writes=[("acc", tt, n)])
                    else:
                        T.op("dve", lambda e: e.scalar_tensor_tensor(out=a, in0=ps[:], scalar=rw[:, tt, ex:ex + 1], in1=a, op0=ALU.mult, op1=ALU.add),
                             reads=[pk, "rw"], writes=[("acc", tt, n)])

                expert_dense(T, C, hT, "hT", wg, wgk, wu, wuk, wd, wdk, epi)
            for tt in range(8):
                T.dma("sp", part[r * NT + tt * 128:r * NT + (tt + 1) * 128, :], acc[:, tt, :],
                      reads=[("acc", tt, n) for n in range(4)], writes=[("part", r, tt)], dsem="o%d" % tt)
        T.emit()
    return nc


def build_l3():
    nc = bass.Bass("TRN2", target_bir_lowering=False)
    parts = nc.dram_tensor("parts", [NCORES * NT, D], F32, kind="ExternalInput").ap()
    x1s = nc.dram_tensor("x1s", [NT, D], F32, kind="ExternalInput").ap()
    g2row = nc.dram_tensor("g2row", [1, D], F32, kind="ExternalInput").ap()
    y = nc.dram_tensor("y", [NT, D], F32, kind="ExternalOutput").ap()
    with ExitStack() as st:
        T = Tracker(nc, st)
        sb = lambda n, s, d: st.enter_context(nc.sbuf_tensor(n, s, d))
        g2bc = sb("g2bc", [128, D], F32)
        pb = [sb("pb%d" % i, [128, 8, D], F32) for i in range(2)]
        xb = [sb("xb%d" % i, [128, D], F32) for i in range(2)]
        T.dma("sp", g2bc[:], g2row.partition_broadcast(128), writes=["g2bc"], dsem="c")
        for tt in range(8):
            b = tt % 2
            T.dma("sp", xb[b][:], x1s[tt * 128:(tt + 1) * 128, :], writes=["xb%d" % b], dsem="q%d" % b)
            for c in range(NCORES):
                T.dma("sp", pb[b][:, c, :], parts[c * NT + tt * 128:c * NT + (tt + 1) * 128, :], writes=["pb%d" % b], dsem="p%d" % b)
            for c in range(1, NCORES):
                T.op("dve", lambda e, b=b, c=c: e.tensor_tensor(out=pb[b][:, 0, :], in0=pb[b][:, 0, :], in1=pb[b][:, c, :], op=ALU.add),
                     reads=["pb%d" % b], writes=["pb%d" % b])
            T.op("dve", lambda e, b=b: e.tensor_tensor(out=pb[b][:, 0, :], in0=pb[b][:, 0, :], in1=g2bc[:], op=ALU.mult),
                 reads=["pb%d" % b, "g2bc"], writes=["pb%d" % b])
            T.op("dve", lambda e, b=b: e.tensor_tensor(out=xb[b][:], in0=xb[b][:], in1=pb[b][:, 0, :], op=ALU.add),
                 reads=["pb%d" % b, "xb%d" % b], writes=["xb%d" % b])
            T.dma("sp", y[tt * 128:(tt + 1) * 128, :], xb[b][:], reads=["xb%d" % b], writes=[("y", tt)], dsem="o%d" % b)
        T.emit()
    return nc


def _consts(h):
    ident = np.eye(128, dtype=np.float32)
    gb = np.zeros((128, 4, 8, 8), np.float32)
    for jj in range(4):
        j = 4 + jj
        gb[:, jj, :, j:] = -1e30
        if h == 0:
            gb[:, jj, :, 0:4] = -1e30
    es = np.zeros((64, 64, 128), np.float32)
    for r in range(64):
        es[r, r, :] = 1.0
    cm = np.zeros((128, 4, 512), np.float32)
    k = np.arange(128)[:, None]
    q = np.arange(512)[None, :]
    for r in range(4):
        same = (q // 256) == (r // 2)
        cm[:, r, :] = np.where(same & ((r * 128 + k) > q), NEG, 0.0)
    return ident, gb.reshape(128, 256), es.reshape(64, 8192), cm.reshape(128, 2048)


def _pp(v):
    return np.ascontiguousarray(np.asarray(v, np.float32).reshape(-1, 128).T)


def kernel_unfused(x, c, w_ada, b_ada, norm1_g, w_in, q_norm_g, k_norm_g, conv_w, attn_out_g, conv_out_g, w_o,
           norm2_g, w_router, router_bias, w_gate, w_up, w_down, ws_gate, ws_up, ws_down):
    f = lambda a: np.ascontiguousarray(np.asarray(a, dtype=np.float32))
    x = f(x); c = f(c)
    cores = list(range(NCORES))
    cT = np.ascontiguousarray(c.reshape(4, 16, 128).transpose(2, 1, 0).reshape(128, 64))
    wa = f(w_ada)[0]
    ba = f(b_ada)
    in0 = [{"cT": cT, "wa": np.ascontiguousarray(wa[:, k * 1536:(k + 1) * 1536]),
            "ba": np.ascontiguousarray(ba[:, k * 1536:(k + 1) * 1536])} for k in cores]
    r0 = run_bass_kernel_spmd(build_l0(), in0, core_ids=cores)
    mod = np.concatenate([r0.results[k]["mod"] for k in cores], axis=1)
    w_in0 = f(w_in)[0]; w_o0 = f(w_o)[0]; w_r0 = f(w_router)[0]
    wsg = f(ws_gate)[0]; wsu = f(ws_up)[0]; wsd = f(ws_down)[0]
    ngT = np.concatenate([_pp(f(norm1_g)[0]), _pp(f(norm2_g)[0])], axis=1)
    cw = f(conv_w)[0]
    in1 = []
    for k in cores:
        b, h = k // 2, k % 2
        ident, gb, es, cm = _consts(h)
        small = np.zeros((128, 64), np.float32)
        small[:, 0] = f(q_norm_g)[0]
        small[:, 1] = f(k_norm_g)[0]
        small[:, 2:26] = cw.reshape(3, 8, 128).transpose(2, 1, 0).reshape(128, 24)
        small[:, 26:34] = _pp(f(attn_out_g)[0])
        small[:, 34:42] = _pp(f(conv_out_g)[0])
        small[:, 42] = float(h)
        in1.append({
            "xo": np.ascontiguousarray(x[b, h * NT:(h + 1) * NT]), "xp": np.ascontiguousarray(x[b, 0:NT]),
            "modT": _pp(mod[b]), "g1row": np.ascontiguousarray(mod[b:b + 1, 2 * D:3 * D]),
            "g2row": np.ascontiguousarray(mod[b:b + 1, 5 * D:6 * D]), "ngT": ngT, "w_in": w_in0, "smallT": small,
            "w_o": w_o0, "w_r": w_r0, "rbias": f(router_bias), "wsg": wsg, "wsu": wsu, "wsd": wsd,
            "ident": ident, "gbias": gb, "esel": es, "cmask": cm})
    r1 = run_bass_kernel_spmd(build_l1(), in1, core_ids=cores)
    hfa = np.concatenate([r1.results[k]["o_hfT"] for k in cores], axis=0)
    rw_all = np.concatenate([r1.results[k]["o_rw"] for k in cores], axis=0)
    wg = f(w_gate)[0]; wu = f(w_up)[0]; wd = f(w_down)[0]
    in2 = [{"hfa": hfa, "rwl": np.ascontiguousarray(rw_all[:, 32 * k:32 * (k + 1)]),
            "wg": wg[32 * k:32 * (k + 1)].reshape(32 * D, 512), "wu": wu[32 * k:32 * (k + 1)].reshape(32 * D, 512),
            "wd": wd[32 * k:32 * (k + 1)].reshape(32 * 512, D)} for k in cores]
    r2 = run_bass_kernel_spmd(build_l2(), in2, core_ids=cores)
    in3 = []
    for k in cores:
        b = k // 2
        parts = np.concatenate([r2.results[cc]["part"][k * NT:(k + 1) * NT] for cc in cores], axis=0)
        in3.append({"parts": parts, "x1s": r1.results[k]["o_x1s"], "g2row": np.ascontiguousarray(mod[b:b + 1, 5 * D:6 * D])})
    r3 = run_bass_kernel_spmd(build_l3(), in3, core_ids=cores)
    out = np.stack([np.concatenate([r3.results[2 * b]["y"], r3.results[2 * b + 1]["y"]], axis=0) for b in range(NB)], axis=0)
    return out.astype(np.float32)


def fused_inputs(x, c, w_ada, b_ada, norm1_g, w_in, q_norm_g, k_norm_g, conv_w, attn_out_g, conv_out_g, w_o,
                 norm2_g, w_router, router_bias, w_gate, w_up, w_down, ws_gate, ws_up, ws_down, cores, ne=256):
    f = lambda a: np.ascontiguousarray(np.asarray(a, dtype=np.float32))
    x = f(x); c = f(c)
    wa = f(w_ada)[0]; ba = f(b_ada)
    w_in0 = f(w_in)[0]; w_o0 = f(w_o)[0]; w_r0 = f(w_router)[0]
    wsg = f(ws_gate)[0]; wsu = f(ws_up)[0]; wsd = f(ws_down)[0]
    nex = max(ne, 1)
    wg = f(w_gate)[0][:nex].reshape(nex * D, 512)
    wu = f(w_up)[0][:nex].reshape(nex * D, 512)
    wd = f(w_down)[0][:nex].reshape(nex * 512, D)
    ngT = np.concatenate([_pp(f(norm1_g)[0]), _pp(f(norm2_g)[0])], axis=1)
    cw = f(conv_w)[0]
    baT = _pp(ba[0])
    rb = f(router_bias)
    ins = []
    for k in cores:
        b, h = k // 2, k % 2
        ident, gb, es, cm = _consts(h)
        small = np.zeros((128, 64), np.float32)
        small[:, 0] = f(q_norm_g)[0]
        small[:, 1] = f(k_norm_g)[0]
        small[:, 2:26] = cw.reshape(3, 8, 128).transpose(2, 1, 0).reshape(128, 24)
        small[:, 26:34] = _pp(f(attn_out_g)[0])
        small[:, 34:42] = _pp(f(conv_out_g)[0])
        small[:, 42] = float(h)
        ins.append({
            "xo": np.ascontiguousarray(x[b, h * NT:(h + 1) * NT]), "xp": np.ascontiguousarray(x[b, 0:NT]),
            "cT": _pp(c[b]), "w_ada": wa, "baT": baT, "b_ada": ba, "ngT": ngT, "w_in": w_in0, "smallT": small,
            "w_o": w_o0, "w_r": w_r0, "rbias": rb, "wsg": wsg, "wsu": wsu, "wsd": wsd,
            "ident": ident, "gbias": gb, "esel": es, "cmask": cm, "wg": wg, "wu": wu, "wd": wd})
    return ins


def kernel(**inputs):
    cores = list(range(NCORES))
    ins = fused_inputs(cores=cores, **inputs)
    res = run_bass_kernel_spmd(build_fused(), ins, core_ids=cores)
    out = np.stack([np.concatenate([res.results[2 * b]["y"], res.results[2 * b + 1]["y"]], axis=0) for b in range(NB)], axis=0)
    return out.astype(np.float32)
```

```python
from contextlib import ExitStack

import numpy as np
import ml_dtypes
import concourse.bass as bass
import concourse.mybir as mybir
from concourse.bass_utils import run_bass_kernel_spmd

F32 = mybir.dt.float32
BF16 = mybir.dt.bfloat16
AF = mybir.ActivationFunctionType
ALU = mybir.AluOpType
AX = mybir.AxisListType

NCORES = 8
D = 2048
S = 2048
NB = 4
NT = 1024
EPS = 1e-6
NEG = -30000.0


class _Eng:
    def __init__(self, name, sem):
        self.name = name
        self.sem = sem
        self.sig = 0
        self.pending = []
        self.waited = {}
        self.prog = []


class Tracker:
    ENG = ("pe", "act", "dve", "pool", "sp")

    def __init__(self, nc, stack):
        self.nc = nc
        self.stack = stack
        self.E = {n: _Eng(n, stack.enter_context(nc.semaphore("sem_" + n))) for n in self.ENG}
        self.dsem = {}
        self.last_w = {}
        self.readers = {}

    def _dsem(self, name):
        if name not in self.dsem:
            self.dsem[name] = [self.stack.enter_context(self.nc.semaphore("d_" + name)), 0]
        return self.dsem[name]

    def _deps(self, reads, writes):
        deps = []
        for k in reads:
            t = self.last_w.get(k)
            if t is not None:
                deps.append(t)
        for k in writes:
            t = self.last_w.get(k)
            if t is not None:
                deps.append(t)
            deps.extend(self.readers.get(k, ()))
        return deps

    def _wait(self, E, deps):
        for t in deps:
            if t[0] == "e":
                if t[1] == "pe" and E.name == "pe":
                    continue
                v = t[2]
                assert v is not None, "dependency on unsignaled op (%s)" % t[1]
                key = ("e", t[1])
                sem = self.E[t[1]].sem
            else:
                v = t[2]
                key = ("d", t[1])
                sem = self.dsem[t[1]][0]
            if E.waited.get(key, 0) >= v:
                continue
            E.waited[key] = v
            E.prog.append(("wait", sem, v))

    def _record(self, tok, reads, writes):
        for k in reads:
            self.readers.setdefault(k, []).append(tok)
        for k in writes:
            self.last_w[k] = tok
            self.readers[k] = []

    def op(self, en, fn, reads=(), writes=(), sig=True):
        E = self.E[en]
        self._wait(E, self._deps(reads, writes))
        tok = ["e", en, None]
        if sig:
            E.sig += 1
            tok[2] = E.sig
            for p in E.pending:
                p[2] = E.sig
            E.pending = []
        else:
            E.pending.append(tok)
        E.prog.append(("op", fn, sig))
        self._record(tok, reads, writes)

    def dma(self, q, out, in_, reads=(), writes=(), dsem="misc"):
        E = self.E[q]
        self._wait(E, self._deps(reads, writes))
        Dm = self._dsem(dsem)
        Dm[1] += 16
        tok = ["d", dsem, Dm[1]]
        E.prog.append(("dma", out, in_, Dm[0]))
        self._record(tok, reads, writes)

    def barrier(self):
        for E in self.E.values():
            assert not E.pending, "pending unsignaled ops on %s at barrier" % E.name
        for E in self.E.values():
            for O in self.E.values():
                if O is E or O.sig == 0:
                    continue
                if E.waited.get(("e", O.name), 0) < O.sig:
                    E.waited[("e", O.name)] = O.sig
                    E.prog.append(("wait", O.sem, O.sig))
            for dn, Dm in self.dsem.items():
                if Dm[1] and E.waited.get(("d", dn), 0) < Dm[1]:
                    E.waited[("d", dn)] = Dm[1]
                    E.prog.append(("wait", Dm[0], Dm[1]))
        self.last_w = {}
        self.readers = {}

    def emit(self):
        nc = self.nc
        self.barrier()
        with nc.Block() as block:
            def run(E, eng):
                for it in E.prog:
                    if it[0] == "wait":
                        eng.wait_ge(it[1], it[2])
                    elif it[0] == "op":
                        ins = it[1](eng)
                        if it[2]:
                            ins.then_inc(E.sem, 1)
                    else:
                        eng.dma_start(out=it[1], in_=it[2]).then_inc(it[3], 16)

            @block.tensor
            def _(eng):
                run(self.E["pe"], eng)

            @block.scalar
            def _(eng):
                run(self.E["act"], eng)

            @block.vector
            def _(eng):
                run(self.E["dve"], eng)

            @block.gpsimd
            def _(eng):
                run(self.E["pool"], eng)

            @block.sync
            def _(eng):
                run(self.E["sp"], eng)


def _wview(w2d):
    return w2d.rearrange("(kc p) n -> p kc n", p=128)


def build_l0():
    nc = bass.Bass("TRN2", target_bir_lowering=False)
    cT = nc.dram_tensor("cT", [128, 64], F32, kind="ExternalInput").ap()
    wa = nc.dram_tensor("wa", [D, 1536], F32, kind="ExternalInput").ap()
    ba = nc.dram_tensor("ba", [1, 1536], F32, kind="ExternalInput").ap()
    mod = nc.dram_tensor("mod", [4, 1536], F32, kind="ExternalOutput").ap()
    with ExitStack() as st:
        T = Tracker(nc, st)
        sb = lambda n, s, d: st.enter_context(nc.sbuf_tensor(n, s, d))
        c_f = sb("c_f", [128, 64], F32)
        c_b = sb("c_b", [128, 64], BF16)
        wt = sb("wt", [128, 16, 1536], BF16)
        bt = sb("bt", [4, 1536], F32)
        ot = sb("ot", [4, 1536], F32)
        ps = st.enter_context(nc.psum_tensor("ps", [128, 512], F32))
        T.dma("sp", c_f[:], cT, writes=["c_f"], dsem="a")
        T.dma("sp", bt[:], ba.partition_broadcast(4), writes=["bt"], dsem="b")
        T.dma("pool", wt[:], _wview(wa), writes=["wt"], dsem="w")
        T.op("act", lambda e: e.activation(out=c_b[:], in_=c_f[:], func=AF.Silu), reads=["c_f"], writes=["c_b"])
        for n in range(3):
            for kc in range(16):
                T.op("pe", lambda e, kc=kc, n=n: e.matmul(ps[0:4, :], lhsT=c_b[:, kc * 4:(kc + 1) * 4],
                                                          rhs=wt[:, kc, n * 512:(n + 1) * 512],
                                                          start=(kc == 0), stop=(kc == 15)),
                     reads=["c_b", "wt"], writes=["ps"], sig=(kc == 15))
            T.op("dve", lambda e, n=n: e.tensor_tensor(out=ot[:, n * 512:(n + 1) * 512], in0=ps[0:4, :],
                                                       in1=bt[:, n * 512:(n + 1) * 512], op=ALU.add),
                 reads=["ps", "bt"], writes=["ot"])
        T.dma("sp", mod, ot[:], reads=["ot"], writes=["mod"], dsem="o")
        T.emit()
    return nc


class Ctx:
    pass


def rms_T_a(T, C, src, src_key):
    i = getattr(C, "rms_i", 0)
    C.rms_i = i + 1
    sqbs = getattr(C, "sqbs", None) or [(C.sqb, "sqb")]
    rss = getattr(C, "rss", None) or [(C.rs, "rs")]
    psns = getattr(C, "psns", None) or [(C.ps_n, "ps_n")]
    sqb, sqk = sqbs[i % len(sqbs)]
    rs, rsk = rss[i % len(rss)]
    pn, pnk = psns[i % len(psns)]
    T.op("act", lambda e: e.activation(out=sqb[:], in_=src, func=AF.Square), reads=[src_key], writes=[sqk])
    return (sqb, sqk, rs, rsk, pn, pnk)


def rms_T_b(T, C, st, src, src_key, gain, out_ap, out_key, n_feat):
    sqb, sqk, rs, rsk, pn, pnk = st
    T.op("pe", lambda e: e.matmul(pn[:], lhsT=C.ones_b[:], rhs=sqb[:], start=True, stop=True),
         reads=[sqk], writes=[pnk])
    T.op("act", lambda e: e.activation(out=rs[:], in_=pn[:], func=AF.Sqrt, bias=C.eps_t[:, 0:1], scale=1.0 / n_feat),
         reads=[pnk], writes=[rsk])
    T.op("dve", lambda e: e.reciprocal(out=rs[:], in_=rs[:]), reads=[rsk], writes=[rsk])
    T.op("dve", lambda e: e.scalar_tensor_tensor(out=out_ap, in0=src, scalar=gain, in1=rs[:], op0=ALU.mult, op1=ALU.mult),
         reads=[src_key, rsk], writes=[out_key])


def rms_T(T, C, src, src_key, gain, out_ap, out_key, n_feat):
    st = rms_T_a(T, C, src, src_key)
    rms_T_b(T, C, st, src, src_key, gain, out_ap, out_key, n_feat)


def norm_transpose(T, C, xt, xt_key, i, AT, shT, dst, tok0, tag):
    ssi = C.ss[:, i:i + 1]
    junk = C.junk
    T.op("dve", lambda e: e.memset(C.ss4[:], 0.0), writes=["ss4"])
    for q4 in range(4):
        T.op("act", lambda e, q4=q4: e.activation(out=junk[:, q4 * 512:(q4 + 1) * 512], in_=xt[:, q4 * 512:(q4 + 1) * 512], func=AF.Square,
                                                  accum_out=C.ss4[:, q4:q4 + 1]),
             reads=[xt_key, "ss4"], writes=["junk", "ss4"])
    T.op("dve", lambda e: e.tensor_reduce(out=ssi, in_=C.ss4[:], axis=AX.X, op=ALU.add), reads=["ss4"], writes=[("ss", tag, i)])
    T.op("act", lambda e: e.activation(out=ssi, in_=ssi, func=AF.Sqrt, bias=C.eps_t[:, 0:1], scale=1.0 / D),
         reads=[("ss", tag, i)], writes=[("ss", tag, i)])
    T.op("dve", lambda e: e.reciprocal(out=ssi, in_=ssi), reads=[("ss", tag, i)], writes=[("ss", tag, i)])
    xn = C.xn[i % 2]
    xk = "xn%d" % ((i % 2) if C.xn[0] is not C.xn[1] else 0)
    T.op("act", lambda e: e.activation(out=xn[:], in_=xt, func=AF.Copy, scale=ssi), reads=[xt_key, ("ss", tag, i)], writes=[xk])
    for g in range(4):
        pb = C.psb[g % 2]
        pk = "psb%d" % (g % 2)
        for j in range(4):
            dc = g * 4 + j
            T.op("pe", lambda e, dc=dc, j=j, pb=pb: e.transpose(out=pb[:, j * 128:(j + 1) * 128], in_=xn[:, dc * 128:(dc + 1) * 128],
                                                                identity=C.ident_b[:]),
                 reads=[xk, "ident_b"], writes=[pk], sig=(j == 3))
        for j in range(4):
            dc = g * 4 + j
            T.op("dve", lambda e, dc=dc, j=j, pb=pb: e.tensor_scalar(out=dst[:, dc, tok0:tok0 + 128], in0=pb[:, j * 128:(j + 1) * 128],
                                                                    scalar1=AT[:, dc:dc + 1], scalar2=shT[:, dc:dc + 1],
                                                                    op0=ALU.mult, op1=ALU.add),
                 reads=[pk, "modv"], writes=[(tag, dc, i)])


def expert_dense(T, C, hT, hkey, wg, wgk, wu, wuk, wd, wdk, epi):
    for th in range(2):
        for fc in range(4):
            for (w, wk, ps, pk) in ((wg, wgk, C.ps_g, "ps_g"), (wu, wuk, C.ps_u, "ps_u")):
                for kc in range(16):
                    T.op("pe", lambda e, w=w, ps=ps, kc=kc, fc=fc, th=th: e.matmul(
                        ps[:], lhsT=w[:, kc, fc * 128:(fc + 1) * 128], rhs=hT[:, kc, th * 512:(th + 1) * 512],
                        start=(kc == 0), stop=(kc == 15)),
                        reads=[wk, hkey], writes=[pk], sig=(kc == 15))
            T.op("act", lambda e: e.activation(out=C.sg[:], in_=C.ps_g[:], func=AF.Silu), reads=["ps_g"], writes=["sg"])
            T.op("dve", lambda e, fc=fc, th=th: e.tensor_tensor(out=C.hid[:, fc, th * 512:(th + 1) * 512], in0=C.sg[:], in1=C.ps_u[:],
                                                               op=ALU.mult),
                 reads=["sg", "ps_u"], writes=[("hid", fc, th)])
    cnt = 0
    for tt in range(8):
        for n in range(4):
            ps = C.ps_d[cnt % 2]
            pk = "ps_d%d" % (cnt % 2)
            cnt += 1
            for fc in range(4):
                T.op("pe", lambda e, ps=ps, fc=fc, tt=tt, n=n: e.matmul(
                    ps[:], lhsT=C.hid[:, fc, tt * 128:(tt + 1) * 128], rhs=wd[:, fc, n * 512:(n + 1) * 512],
                    start=(fc == 0), stop=(fc == 3)),
                    reads=[wdk, ("hid", fc, tt // 4)], writes=[pk], sig=(fc == 3))
            epi(tt, n, ps, pk)


def expert_dense2(T, C, hT, hkey, wg, wgk, wu, wuk, wd, wdk, g2bc, scal_of_tt, scal_key, x1, pbanks, cnt):
    unit = 0
    for th in range(2):
        for fc in range(4):
            for (w, wk, ps, pk) in ((wg, wgk, C.ps_g, "ps_g"), (wu, wuk, C.ps_u, "ps_u")):
                for kc in range(16):
                    T.op("pe", lambda e, w=w, ps=ps, kc=kc, fc=fc, th=th: e.matmul(
                        ps[:], lhsT=w[:, kc, fc * 128:(fc + 1) * 128], rhs=hT[:, kc, th * 512:(th + 1) * 512],
                        start=(kc == 0), stop=(kc == 15)),
                        reads=[wk, hkey], writes=[pk], sig=(kc == 15))
            T.op("act", lambda e: e.activation(out=C.sg[:], in_=C.ps_g[:], func=AF.Silu), reads=["ps_g"], writes=["sg"])
            T.op("dve", lambda e, fc=fc, th=th: e.tensor_tensor(out=C.hid[:, fc, th * 512:(th + 1) * 512], in0=C.sg[:], in1=C.ps_u[:],
                                                               op=ALU.mult),
                 reads=["sg", "ps_u"], writes=[("hid", fc, th)])
            if 2 <= unit < 6:
                j = unit - 2
                T.op("dve", lambda e, j=j: e.tensor_tensor(out=wd[:, j, :], in0=wd[:, j, :], in1=g2bc[:], op=ALU.mult),
                     reads=[wdk, "g2bc"], writes=[wdk])
            unit += 1
    for tt in range(8):
        scal = scal_of_tt(tt)
        for n in range(4):
            ps, pk = pbanks[cnt[0] % len(pbanks)]
            cnt[0] += 1
            for fc in range(4):
                T.op("pe", lambda e, ps=ps, fc=fc, tt=tt, n=n: e.matmul(
                    ps[:], lhsT=C.hid[:, fc, tt * 128:(tt + 1) * 128], rhs=wd[:, fc, n * 512:(n + 1) * 512],
                    start=(fc == 0), stop=(fc == 3)),
                    reads=[wdk, ("hid", fc, tt // 4)], writes=[pk], sig=(fc == 3))
            xs = x1[:, tt, n * 512:(n + 1) * 512]
            T.op("dve", lambda e, ps=ps, xs=xs, scal=scal: e.scalar_tensor_tensor(out=xs, in0=ps[:], scalar=scal, in1=xs,
                                                                                 op0=ALU.mult, op1=ALU.add),
                 reads=[pk, scal_key, ("x1", tt, n)], writes=[("x1", tt, n)])


def build_l1(debug=False):
    nc = bass.Bass("TRN2", target_bir_lowering=False)
    din = lambda n, s, d=F32: nc.dram_tensor(n, s, d, kind="ExternalInput").ap()
    xo = din("xo", [NT, D])
    xp = din("xp", [NT, D])
    modT = din("modT", [128, 96])
    g1row = din("g1row", [1, D])
    g2row = din("g2row", [1, D])
    ngT = din("ngT", [128, 32])
    w_in = din("w_in", [D, 6144])
    smallT = din("smallT", [128, 64])
    w_o = din("w_o", [D, D])
    w_r = din("w_r", [D, 256])
    rbias = din("rbias", [1, 256])
    wsg = din("wsg", [D, 512])
    wsu = din("wsu", [D, 512])
    wsd = din("wsd", [512, D])
    ident = din("ident", [128, 128])
    gbias = din("gbias", [128, 4 * 64])
    esel = din("esel", [64, 64 * 128])
    cmask = din("cmask", [128, 4 * 512])
    o_hfT = nc.dram_tensor("o_hfT", [D, NT], BF16, kind="ExternalOutput").ap()
    o_x1s = nc.dram_tensor("o_x1s", [NT, D], F32, kind="ExternalOutput").ap()
    o_rw = nc.dram_tensor("o_rw", [NT, 256], F32, kind="ExternalOutput").ap()

    with ExitStack() as st:
        T = Tracker(nc, st)
        C = Ctx()
        sb = lambda n, s, d, stack=st: stack.enter_context(nc.sbuf_tensor(n, s, d))
        psf = lambda n: st.enter_context(nc.psum_tensor(n, [128, 512], F32))
        C.ps_g = psf("ps_g"); C.ps_u = psf("ps_u"); C.ps_n = psf("ps_n")
        C.ps_d = [psf("ps_d0"), psf("ps_d1")]
        C.ps_x = psf("ps_x")
        C.psb = [st.enter_context(nc.psum_tensor("psb%d" % i, [128, 1024], BF16)) for i in range(2)]
        ident_f = sb("ident_f", [128, 128], F32)
        C.ident_b = sb("ident_b", [128, 128], BF16)
        C.ones_b = sb("ones_b", [128, 128], BF16)
        C.eps_t = sb("eps_t", [128, 1], F32)
        modv = sb("modv", [128, 96], F32)
        ng = sb("ng", [128, 32], F32)
        AT = sb("AT", [128, 32], F32)
        sm = sb("sm", [128, 64], F32)
        C.ss = sb("ss", [128, 32], F32)
        C.ss4 = sb("ss4", [128, 4], F32)
        C.sqb = sb("sqb", [128, 512], BF16)
        C.rs = sb("rs", [128, 512], F32)
        C.sg = sb("sg", [128, 512], F32)

        T.dma("sp", ident_f[:], ident, writes=["ident_f"], dsem="c0a")
        T.dma("sp", modv[:], modT, writes=["modv0"], dsem="c0b")
        T.dma("sp", ng[:], ngT, writes=["ng"], dsem="c0c")
        T.dma("sp", sm[:], smallT, writes=["sm"], dsem="c0d")
        T.op("dve", lambda e: e.tensor_copy(out=C.ident_b[:], in_=ident_f[:]), reads=["ident_f"], writes=["ident_b"])
        T.op("pool", lambda e: e.memset(C.ones_b[:], 1.0), writes=["ones_b"])
        T.op("pool", lambda e: e.memset(C.eps_t[:], EPS), writes=["eps_t"])
        T.op("dve", lambda e: e.tensor_scalar(out=AT[:, 0:16], in0=modv[:, 16:32], scalar1=1.0, scalar2=None, op0=ALU.add),
             reads=["modv0"], writes=["AT"])
        T.op("dve", lambda e: e.tensor_scalar(out=AT[:, 16:32], in0=modv[:, 64:80], scalar1=1.0, scalar2=None, op0=ALU.add),
             reads=["modv0"], writes=["AT"])
        T.op("dve", lambda e: e.tensor_tensor(out=AT[:], in0=AT[:], in1=ng[:], op=ALU.mult), reads=["AT", "ng"], writes=["AT"])
        T.barrier()
        A1T = AT[:, 0:16]; A2T = AT[:, 16:32]
        sh1T = modv[:, 0:16]; sh2T = modv[:, 48:64]
        qg = sm[:, 0:1]; kg = sm[:, 1:2]
        convw = lambda g, j: sm[:, 2 + g * 3 + j:3 + g * 3 + j]
        aog = lambda h: sm[:, 26 + h:27 + h]
        cog = lambda g: sm[:, 34 + g:35 + g]
        halo = sm[:, 42:43]

        yT = sb("yT", [128, 8, NT], BF16)
        aT = sb("aT", [128, 8, NT], BF16)
        with ExitStack() as sA:
            sbA = lambda n, s, d: sb(n, s, d, sA)
            QT = sbA("QT", [128, 8, NT], BF16)
            KT = sbA("KT", [128, 8, 2048], BF16)
            V = sbA("V", [128, 16, 1024], BF16)
            with ExitStack() as sP:
                sbP = lambda n, s, d: sb(n, s, d, sP)
                hT = sbP("hT", [128, 16, 2048], BF16)
                with ExitStack() as sX:
                    xb = [sb("xb%d" % i, [128, 2048], F32, sX) for i in range(2)]
                    C.junk = aT[:, 0:2, :].rearrange("p h t -> p (h t)")
                    _xn = sb("xn0", [128, 2048], BF16, sX)
                    C.xn = [_xn, _xn]
                    for i in range(16):
                        src = xp if i < 8 else xo
                        j = i % 8
                        T.dma("sp", xb[i % 2][:], src[j * 128:(j + 1) * 128, :], writes=["xb%d" % (i % 2)], dsem="x%d" % (i % 2))
                        norm_transpose(T, C, xb[i % 2][:], "xb%d" % (i % 2), i, A1T, sh1T, hT, i * 128, "hT")
                    T.barrier()
                wt = aT[:].rearrange("p h t -> p (h t)").rearrange("p (kc n) -> p kc n", n=512)
                if debug:
                    o_h = nc.dram_tensor("o_h", [128, 16 * 2048], BF16, kind="ExternalOutput").ap()
                    T.dma("sp", o_h, hT[:].rearrange("p c t -> p (c t)"), writes=["o_h"], dsem="dbg2")
                    T.barrier()
                uz = sbP("uz", [128, 8, NT + 2], BF16)
                zc = sbP("zc", [128, 512], F32)
                for n in (0, 1, 2, 3, 4, 5, 6, 7, 10, 11):
                    T.dma("pool", wt, _wview(w_in[:, n * 512:(n + 1) * 512]), writes=["wt"], dsem="wt")
                    kind = n // 2
                    for hh in range(4):
                        hd = (n % 2) * 4 + hh
                        if kind in (0, 1):
                            chunks = [(1024 + 512 * t, 512 * t) for t in range(2)] if kind == 0 else [(512 * t, 512 * t) for t in range(4)]
                            for (c0, o0) in chunks:
                                for kc in range(16):
                                    T.op("pe", lambda e, kc=kc, hh=hh, c0=c0: e.matmul(
                                        C.ps_x[:], lhsT=wt[:, kc, hh * 128:(hh + 1) * 128], rhs=hT[:, kc, c0:c0 + 512],
                                        start=(kc == 0), stop=(kc == 15)), reads=["wt"], writes=["ps_x"], sig=(kc == 15))
                                dst = (QT if kind == 0 else KT)[:, hd, o0:o0 + 512]
                                rms_T(T, C, C.ps_x[:], "ps_x", qg if kind == 0 else kg, dst, ("qk", kind, hd, o0), 128)
                        elif kind in (3, 4, 5):
                            for t in range(2):
                                for kc in range(16):
                                    T.op("pe", lambda e, kc=kc, hh=hh, t=t: e.matmul(
                                        C.ps_x[:], lhsT=wt[:, kc, hh * 128:(hh + 1) * 128], rhs=hT[:, kc, 1024 + 512 * t:1536 + 512 * t],
                                        start=(kc == 0), stop=(kc == 15)), reads=["wt"], writes=["ps_x"], sig=(kc == 15))
                                sl = uz[:, hd, 2 + 512 * t:2 + 512 * (t + 1)]
                                if kind == 3:
                                    T.op("act", lambda e, sl=sl: e.activation(out=sl, in_=C.ps_x[:], func=AF.Copy),
                                         reads=["ps_x"], writes=[("uz", hd, t)])
                                elif kind == 5:
                                    T.op("dve", lambda e, sl=sl: e.tensor_tensor(out=sl, in0=C.ps_x[:], in1=sl, op=ALU.mult),
                                         reads=["ps_x", ("uz", hd, t)], writes=[("uz", hd, t)])
                                else:
                                    pass
                            if kind in (3, 5):
                                for kc in range(16):
                                    T.op("pe", lambda e, kc=kc, hh=hh: e.matmul(
                                        C.ps_x[:, 0:2], lhsT=wt[:, kc, hh * 128:(hh + 1) * 128], rhs=hT[:, kc, 1022:1024],
                                        start=(kc == 0), stop=(kc == 15)), reads=["wt"], writes=["ps_x"], sig=(kc == 15))
                                sl = uz[:, hd, 0:2]
                                if kind == 3:
                                    T.op("act", lambda e, sl=sl: e.activation(out=sl, in_=C.ps_x[:, 0:2], func=AF.Copy),
                                         reads=["ps_x"], writes=[("uz", hd, "h")])
                                else:
                                    T.op("dve", lambda e, sl=sl: e.scalar_tensor_tensor(out=sl, in0=C.ps_x[:, 0:2], scalar=halo, in1=sl,
                                                                                      op0=ALU.mult, op1=ALU.mult),
                                         reads=["ps_x", ("uz", hd, "h")], writes=[("uz", hd, "h")])
                        else:
                            pass
                    if kind == 2:
                        for tt in range(16):
                            for kc in range(16):
                                T.op("pe", lambda e, kc=kc, tt=tt: e.matmul(
                                    C.ps_x[:], lhsT=hT[:, kc, tt * 128:(tt + 1) * 128], rhs=wt[:, kc, :],
                                    start=(kc == 0), stop=(kc == 15)), reads=["wt"], writes=["ps_x"], sig=(kc == 15))
                            T.op("act", lambda e, tt=tt, n=n: e.activation(out=V[:, tt, (n % 2) * 512:(n % 2 + 1) * 512], in_=C.ps_x[:], func=AF.Copy),
                                 reads=["ps_x"], writes=[("V", tt, n)])
                T.barrier()
                for n in (8, 9):
                    T.dma("pool", wt, _wview(w_in[:, n * 512:(n + 1) * 512]), writes=["wt"], dsem="wt")
                    for hh in range(4):
                        g = (n % 2) * 4 + hh
                        for t in range(2):
                            for kc in range(16):
                                T.op("pe", lambda e, kc=kc, hh=hh, t=t: e.matmul(
                                    C.ps_x[:], lhsT=wt[:, kc, hh * 128:(hh + 1) * 128], rhs=hT[:, kc, 1024 + 512 * t:1536 + 512 * t],
                                    start=(kc == 0), stop=(kc == 15)), reads=["wt"], writes=["ps_x"], sig=(kc == 15))
                            z0, z1, z2 = [uz[:, g, 2 + 512 * t - sh:2 + 512 * (t + 1) - sh] for sh in range(3)]
                            cz = zc[:, 0:512]
                            T.op("dve", lambda e, g=g, z0=z0: e.tensor_scalar(out=cz, in0=z0, scalar1=convw(g, 2), scalar2=None, op0=ALU.mult),
                                 reads=["uzall"], writes=["zc"])
                            T.op("dve", lambda e, g=g, z1=z1: e.scalar_tensor_tensor(out=cz, in0=z1, scalar=convw(g, 1), in1=cz, op0=ALU.mult, op1=ALU.add),
                                 reads=["zc"], writes=["zc"])
                            T.op("dve", lambda e, g=g, z2=z2: e.scalar_tensor_tensor(out=cz, in0=z2, scalar=convw(g, 0), in1=cz, op0=ALU.mult, op1=ALU.add),
                                 reads=["zc"], writes=["zc"])
                            T.op("dve", lambda e: e.tensor_tensor(out=cz, in0=C.ps_x[:], in1=cz, op=ALU.mult), reads=["ps_x", "zc"], writes=["zc"])
                            rms_T(T, C, cz, "zc", cog(g), yT[:, g, 512 * t:512 * (t + 1)], ("yT", g, t), 128)
                T.barrier()
            with ExitStack() as s3:
                sb3 = lambda n, s, d: sb(n, s, d, s3)
                kmf = sb3("kmf", [128, 64], F32)
                kmb = sb3("kmb", [128, 64], BF16)
                gbt = sb3("gbt", [128, 4 * 64], F32)
                es_f = sb3("es_f", [64, 2048], F32)
                es_b = sb3("es_b", [64, 64 * 128], BF16)
                cm_f = sb3("cm_f", [128, 2048], F32)
                cm_b = sb3("cm_b", [128, 2048], BF16)
                gb = sb3("gb", [128, 64], F32)
                mx = sb3("mx", [128, 64], F32)
                sel = sb3("sel", [128, 64], F32)
                val = sb3("val", [128, 64], F32)
                biasT = sb3("biasT", [64, NT], BF16)
                PT = [sb3("PT%d" % i, [128, 512], BF16) for i in range(3)]
                rden = sb3("rden", [128, 512], F32)
                a_f = sb3("a_f", [128, 512], F32)
                T.dma("sp", gbt[:], gbias, writes=["gbt"], dsem="c1a")
                for q4 in range(4):
                    T.dma("sp", es_f[:], esel[:, q4 * 2048:(q4 + 1) * 2048], writes=["es_f"], dsem="c1b")
                    T.op("act", lambda e, q4=q4: e.activation(out=es_b[:, q4 * 2048:(q4 + 1) * 2048], in_=es_f[:], func=AF.Copy),
                         reads=["es_f"], writes=[("es_b", q4)])
                T.dma("sp", cm_f[:], cmask, writes=["cm_f"], dsem="c1c")
                T.op("act", lambda e: e.activation(out=cm_b[:], in_=cm_f[:], func=AF.Copy), reads=["cm_f"], writes=["cm_b"])
                for hd in range(8):
                    T.op("dve", lambda e, hd=hd: e.tensor_reduce(out=kmf[:, hd * 8:(hd + 1) * 8],
                                                                 in_=KT[:, hd, :].rearrange("p (n k) -> p n k", k=256),
                                                                 axis=AX.X, op=ALU.add), writes=["kmf"])
                T.op("dve", lambda e: e.tensor_scalar(out=kmb[:], in0=kmf[:], scalar1=1.0 / 256, scalar2=None, op0=ALU.mult),
                     reads=["kmf"], writes=["kmb"])
                for qt in range(8):
                    j = 4 + qt // 2
                    for hd in range(8):
                        T.op("pe", lambda e, hd=hd, qt=qt: e.matmul(C.ps_x[:, hd * 8:(hd + 1) * 8], lhsT=QT[:, hd, qt * 128:(qt + 1) * 128],
                                                                    rhs=kmb[:, hd * 8:(hd + 1) * 8], start=True, stop=True),
                             reads=["kmb"], writes=["ps_x"], sig=(hd == 7))
                    T.op("dve", lambda e, qt=qt: e.tensor_tensor(out=gb[:], in0=C.ps_x[:, 0:64], in1=gbt[:, (qt // 2) * 64:(qt // 2 + 1) * 64], op=ALU.add),
                         reads=["ps_x", "gbt"], writes=["gb"])
                    for hd in range(8):
                        T.op("dve", lambda e, hd=hd: e.max(out=mx[:, hd * 8:(hd + 1) * 8], in_=gb[:, hd * 8:(hd + 1) * 8]),
                             reads=["gb"], writes=[("mx", hd)])
                        T.op("dve", lambda e, hd=hd: e.tensor_scalar(out=sel[:, hd * 8:(hd + 1) * 8], in0=gb[:, hd * 8:(hd + 1) * 8],
                                                                     scalar1=mx[:, hd * 8 + 2:hd * 8 + 3], scalar2=None, op0=ALU.is_ge),
                             reads=["gb", ("mx", hd)], writes=["sel"])
                    T.op("dve", lambda e: e.tensor_scalar(out=val[:], in0=gb[:], scalar1=-1e29, scalar2=None, op0=ALU.is_gt),
                         reads=["gb"], writes=["val"])
                    T.op("dve", lambda e: e.tensor_tensor(out=sel[:], in0=sel[:], in1=val[:], op=ALU.mult),
                         reads=["val", "sel"], writes=["sel"])
                    T.op("dve", lambda e, j=j: e.memset(sel[:].rearrange("p (h n) -> p h n", n=8)[:, :, j:j + 1], 1.0),
                         reads=["sel"], writes=["sel"])
                    T.op("dve", lambda e: e.tensor_scalar(out=sel[:], in0=sel[:], scalar1=-1.0, scalar2=-NEG, op0=ALU.add, op1=ALU.mult),
                         reads=["sel"], writes=["sel"])
                    T.op("pe", lambda e: e.transpose(out=C.ps_n[0:64, 0:128], in_=sel[:], identity=ident_f[:]),
                         reads=["sel"], writes=["ps_n"])
                    T.op("act", lambda e, qt=qt: e.activation(out=biasT[:, qt * 128:(qt + 1) * 128], in_=C.ps_n[0:64, 0:128], func=AF.Copy),
                         reads=["ps_n"], writes=[("biasT", qt)])
                T.barrier()
                pcnt = 0
                for hd in range(8):
                    for qc in range(2):
                        nk = 12 if qc == 0 else 16
                        for kt in range(nk):
                            r = kt - (8 + qc * 4)
                            diag = 0 <= r < 4
                            ps = C.ps_d[pcnt % 2]; pk = "ps_d%d" % (pcnt % 2)
                            pt = PT[pcnt % 3]; ptk = "PT%d" % (pcnt % 3)
                            pcnt += 1
                            T.op("pe", lambda e, ps=ps, hd=hd, kt=kt, qc=qc: e.matmul(
                                ps[:], lhsT=KT[:, hd, kt * 128:(kt + 1) * 128], rhs=QT[:, hd, qc * 512:(qc + 1) * 512], start=True, stop=False),
                                writes=[pk], sig=False)
                            T.op("pe", lambda e, ps=ps, hd=hd, kt=kt, qc=qc, diag=diag: e.matmul(
                                ps[:], lhsT=es_b[:, (hd * 8 + kt // 2) * 128:(hd * 8 + kt // 2 + 1) * 128], rhs=biasT[:, qc * 512:(qc + 1) * 512],
                                start=False, stop=(not diag)), reads=["es_b"], writes=[pk], sig=(not diag))
                            if diag:
                                T.op("pe", lambda e, ps=ps, r=r: e.matmul(ps[:], lhsT=C.ident_b[:], rhs=cm_b[:, r * 512:(r + 1) * 512],
                                                                          start=False, stop=True), reads=["cm_b"], writes=[pk])
                            T.op("act", lambda e, ps=ps, pt=pt: e.activation(out=pt[:], in_=ps[:], func=AF.Exp, scale=128.0 ** -0.5),
                                 reads=[pk], writes=[ptk])
                            T.op("pe", lambda e, pt=pt, hd=hd, kt=kt, nk=nk: e.matmul(
                                C.ps_g[:], lhsT=V[:, kt, hd * 128:(hd + 1) * 128], rhs=pt[:], start=(kt == 0), stop=(kt == nk - 1)),
                                reads=[ptk], writes=["ps_g"], sig=False)
                            T.op("pe", lambda e, pt=pt, kt=kt, nk=nk: e.matmul(
                                C.ps_u[:], lhsT=C.ones_b[:], rhs=pt[:], start=(kt == 0), stop=(kt == nk - 1)),
                                reads=[ptk], writes=["ps_u"])
                        T.op("dve", lambda e: e.reciprocal(out=rden[:], in_=C.ps_u[:]), reads=["ps_u"], writes=["rden"])
                        T.op("dve", lambda e: e.tensor_tensor(out=a_f[:], in0=C.ps_g[:], in1=rden[:], op=ALU.mult),
                             reads=["ps_g", "rden"], writes=["a_f"])
                        rms_T(T, C, a_f[:], "a_f", aog(hd), aT[:, hd, qc * 512:(qc + 1) * 512], ("aT", hd, qc), 128)
                T.barrier()
        if debug:
            o_a = nc.dram_tensor("o_a", [128, 8 * NT], BF16, kind="ExternalOutput").ap()
            o_y = nc.dram_tensor("o_y", [128, 8 * NT], BF16, kind="ExternalOutput").ap()
            T.dma("sp", o_a, aT[:].rearrange("p h t -> p (h t)"), writes=["o_a"], dsem="dbg0")
            T.dma("sp", o_y, yT[:].rearrange("p h t -> p (h t)"), writes=["o_y"], dsem="dbg1")
            T.barrier()
        x1 = sb("x1", [128, 8, D], F32)
        hfT = sb("hfT", [128, 16, NT], BF16)
        with ExitStack() as s4:
            g1bc = sb("g1bc", [128, D], F32, s4)
            wo = sb("wo", [128, 16, 512], BF16, s4)
            tmp = sb("tmp4", [128, 512], F32, s4)
            C.junk = sb("junk4", [128, 2048], BF16, s4)[:]
            C.xn = [sb("xn4_%d" % i, [128, 2048], BF16, s4) for i in range(2)]
            T.dma("sp", g1bc[:], g1row.partition_broadcast(128), writes=["g1bc"], dsem="c2")
            for tt in range(8):
                T.dma("sp", x1[:, tt, :], xo[tt * 128:(tt + 1) * 128, :], writes=[("x1", tt)], dsem="x1_%d" % tt)
            for n in range(4):
                T.dma("pool", wo[:], _wview(w_o[:, n * 512:(n + 1) * 512]), writes=["wo"], dsem="wt")
                for tt in range(8):
                    ps = C.ps_d[tt % 2]; pk = "ps_d%d" % (tt % 2)
                    for kc in range(16):
                        T.op("pe", lambda e, ps=ps, kc=kc, tt=tt: e.matmul(ps[:], lhsT=(aT if kc < 8 else yT)[:, kc % 8, tt * 128:(tt + 1) * 128], rhs=wo[:, kc, :],
                                                                         start=(kc == 0), stop=(kc == 15)),
                             reads=["wo"], writes=[pk], sig=(kc == 15))
                    T.op("dve", lambda e, ps=ps, n=n: e.tensor_tensor(out=tmp[:], in0=ps[:], in1=g1bc[:, n * 512:(n + 1) * 512], op=ALU.mult),
                         reads=[pk, "g1bc"], writes=["tmp4"])
                    T.op("dve", lambda e, tt=tt, n=n: e.tensor_tensor(out=x1[:, tt, n * 512:(n + 1) * 512], in0=x1[:, tt, n * 512:(n + 1) * 512],
                                                                     in1=tmp[:], op=ALU.add),
                         reads=["tmp4", ("x1", tt)], writes=[("x1", tt)])
            T.barrier()
            for tt in range(8):
                norm_transpose(T, C, x1[:, tt, :], ("x1", tt), 16 + tt, A2T, sh2T, hfT, tt * 128, "hfT")
            T.barrier()
        T.dma("sp", o_hfT.rearrange("(kc p) t -> p kc t", p=128), hfT[:], writes=["o_hfT"], dsem="o0")
        with ExitStack() as s5:
            sb5 = lambda n, s, d: sb(n, s, d, s5)
            wr = sb5("wr", [128, 16, 256], BF16)
            rb = sb5("rb", [128, 256], F32)
            sc = sb5("sc", [128, 256], F32)
            ch = sb5("ch", [128, 256], F32)
            mc = sb5("mc", [128, 256], F32)
            m8 = sb5("m8", [128, 64], F32)
            gs = sb5("gs", [128, 8], F32)
            gm = sb5("gm", [128, 8], F32)
            gk = sb5("gk", [128, 8], F32)
            t1 = sb5("t1", [128, 8], F32)
            t8 = sb5("t8", [128, 8], F32)
            den = sb5("den", [128, 1], F32)
            rw = sb5("rw", [128, 8, 256], F32)
            T.dma("pool", wr[:], _wview(w_r), writes=["wr"], dsem="wt")
            T.dma("sp", rb[:], rbias.partition_broadcast(128), writes=["rb"], dsem="c3")
            for tt in range(8):
                for kc in range(16):
                    T.op("pe", lambda e, kc=kc, tt=tt: e.matmul(C.ps_x[:, 0:256], lhsT=hfT[:, kc, tt * 128:(tt + 1) * 128], rhs=wr[:, kc, :],
                                                                start=(kc == 0), stop=(kc == 15)), reads=["wr"], writes=["ps_x"], sig=(kc == 15))
                T.op("act", lambda e: e.activation(out=sc[:], in_=C.ps_x[:, 0:256], func=AF.Sigmoid), reads=["ps_x"], writes=["sc"])
                T.op("dve", lambda e: e.tensor_tensor(out=ch[:], in0=sc[:], in1=rb[:], op=ALU.add), reads=["sc", "rb"], writes=["ch"])
                for g in range(8):
                    T.op("dve", lambda e, g=g: e.max(out=m8[:, g * 8:(g + 1) * 8], in_=ch[:, g * 32:(g + 1) * 32]), reads=["ch"], writes=[("m8", g)])
                m8v = m8[:].rearrange("p (g k) -> p g k", k=8)
                T.op("dve", lambda e: e.tensor_tensor(out=gs[:], in0=m8v[:, :, 0], in1=m8v[:, :, 1], op=ALU.add),
                     reads=[("m8", g) for g in range(8)], writes=["gs"])
                T.op("dve", lambda e: e.max(out=gm[:], in_=gs[:]), reads=["gs"], writes=["gm"])
                T.op("dve", lambda e: e.tensor_scalar(out=gk[:], in0=gs[:], scalar1=gm[:, 3:4], scalar2=None, op0=ALU.is_ge),
                     reads=["gs", "gm"], writes=["gk"])
                T.op("dve", lambda e: e.tensor_scalar(out=t1[:], in0=gk[:], scalar1=-1.0, scalar2=1e30, op0=ALU.add, op1=ALU.mult),
                     reads=["gk"], writes=["t1"])
                for g in range(8):
                    T.op("dve", lambda e, g=g: e.tensor_scalar(out=mc[:, g * 32:(g + 1) * 32], in0=ch[:, g * 32:(g + 1) * 32],
                                                               scalar1=gk[:, g:g + 1], scalar2=t1[:, g:g + 1], op0=ALU.mult, op1=ALU.add),
                         reads=["ch", "gk", "t1"], writes=["mc"])
                T.op("dve", lambda e: e.max(out=t8[:], in_=mc[:]), reads=["mc"], writes=["t8"])
                T.op("dve", lambda e: e.tensor_scalar(out=mc[:], in0=mc[:], scalar1=t8[:, 7:8], scalar2=None, op0=ALU.is_ge),
                     reads=["t8", "mc"], writes=["mc"])
                T.op("dve", lambda e: e.tensor_tensor(out=mc[:], in0=mc[:], in1=sc[:], op=ALU.mult), reads=["mc", "sc"], writes=["mc"])
                T.op("dve", lambda e: e.tensor_reduce(out=den[:], in_=mc[:], axis=AX.X, op=ALU.add), reads=["mc"], writes=["den"])
                T.op("dve", lambda e: e.reciprocal(out=den[:], in_=den[:]), reads=["den"], writes=["den"])
                T.op("dve", lambda e, tt=tt: e.tensor_scalar(out=rw[:, tt, :], in0=mc[:], scalar1=den[:, 0:1], scalar2=2.5, op0=ALU.mult, op1=ALU.mult),
                     reads=["mc", "den"], writes=[("rw", tt)])
            T.dma("sp", o_rw.rearrange("(tt p) e -> p tt e", p=128), rw[:], reads=[("rw", tt) for tt in range(8)], writes=["o_rw"], dsem="o1")
            T.barrier()
        with ExitStack() as s6:
            sb6 = lambda n, s, d: sb(n, s, d, s6)
            wg = sb6("wg", [128, 16, 512], BF16)
            wu = sb6("wu", [128, 16, 512], BF16)
            wd = sb6("wd", [128, 4, D], BF16)
            g2bc = sb6("g2bc", [128, D], F32)
            tmp = sb6("tmp6", [128, 512], F32)
            C.hid = sb6("hid", [128, 4, NT], BF16)
            T.dma("sp", g2bc[:], g2row.partition_broadcast(128), writes=["g2bc"], dsem="c4")
            T.dma("pool", wg[:], _wview(wsg), writes=["wg"], dsem="w6a")
            T.dma("pool", wu[:], _wview(wsu), writes=["wu"], dsem="w6b")
            T.dma("pool", wd[:], _wview(wsd), writes=["wd"], dsem="w6c")

            def epi(tt, n, ps, pk):
                T.op("dve", lambda e: e.tensor_tensor(out=tmp[:], in0=ps[:], in1=g2bc[:, n * 512:(n + 1) * 512], op=ALU.mult),
                     reads=[pk, "g2bc"], writes=["tmp6"])
                T.op("dve", lambda e: e.tensor_tensor(out=x1[:, tt, n * 512:(n + 1) * 512], in0=x1[:, tt, n * 512:(n + 1) * 512], in1=tmp[:], op=ALU.add),
                     reads=["tmp6"], writes=[("x1", tt)])

            expert_dense(T, C, hfT, "hfT", wg, "wg", wu, "wu", wd, "wd", epi)
            for tt in range(8):
                T.dma("sp", o_x1s[tt * 128:(tt + 1) * 128, :], x1[:, tt, :], reads=[("x1", tt)], writes=[("o_x1s", tt)], dsem="o2")
        T.emit()
    return nc


def build_fused(ne=256, debug=False):
    nc = bass.Bass("TRN2", target_bir_lowering=False)
    din = lambda n, s, d=F32: nc.dram_tensor(n, s, d, kind="ExternalInput").ap()
    xo = din("xo", [NT, D])
    xp = din("xp", [NT, D])
    cT = din("cT", [128, 16])
    w_ada = din("w_ada", [D, 6 * D])
    baT = din("baT", [128, 96])
    b_ada = din("b_ada", [1, 6 * D])
    nex = max(ne, 1)
    wg_d = din("wg", [nex * D, 512])
    wu_d = din("wu", [nex * D, 512])
    wd_d = din("wd", [nex * 512, D])
    gscr = nc.dram_tensor("gscr", [256, D], F32).ap()
    ngT = din("ngT", [128, 32])
    w_in = din("w_in", [D, 6144])
    smallT = din("smallT", [128, 64])
    w_o = din("w_o", [D, D])
    w_r = din("w_r", [D, 256])
    rbias = din("rbias", [1, 256])
    wsg = din("wsg", [D, 512])
    wsu = din("wsu", [D, 512])
    wsd = din("wsd", [512, D])
    ident = din("ident", [128, 128])
    gbias = din("gbias", [128, 4 * 64])
    esel = din("esel", [64, 64 * 128])
    cmask = din("cmask", [128, 4 * 512])
    y_out = nc.dram_tensor("y", [NT, D], F32, kind="ExternalOutput").ap()

    with ExitStack() as st:
        T = Tracker(nc, st)
        C = Ctx()
        sb = lambda n, s, d, stack=st: stack.enter_context(nc.sbuf_tensor(n, s, d))
        sbr = lambda n, s, d: st.enter_context(nc.sbuf_tensor(n, s, d, side="right"))
        psf = lambda n: st.enter_context(nc.psum_tensor(n, [128, 512], F32))
        C.ps_g = psf("ps_g"); C.ps_u = psf("ps_u"); C.ps_n = psf("ps_n")
        C.ps_d = [psf("ps_d0"), psf("ps_d1")]
        C.ps_x = psf("ps_x")
        C.psb = [st.enter_context(nc.psum_tensor("psb%d" % i, [128, 1024], BF16)) for i in range(2)]
        ident_f = sb("ident_f", [128, 128], F32)
        C.ident_b = sb("ident_b", [128, 128], BF16)
        C.ones_b = sb("ones_b", [128, 128], BF16)
        C.eps_t = sb("eps_t", [128, 1], F32)
        modv = sb("modv", [128, 96], F32)
        ng = sb("ng", [128, 32], F32)
        AT = sb("AT", [128, 32], F32)
        sm = sb("sm", [128, 64], F32)
        C.ss = sb("ss", [128, 32], F32)
        C.ss4 = sb("ss4", [128, 4], F32)
        C.sqb = sb("sqb", [128, 512], BF16)
        C.rs = sb("rs", [128, 512], F32)
        C.sg = sb("sg", [128, 512], F32)

        T.dma("sp", ident_f[:], ident, writes=["ident_f"], dsem="c0a")
        T.dma("sp", ng[:], ngT, writes=["ng"], dsem="c0c")
        T.dma("sp", sm[:], smallT, writes=["sm"], dsem="c0d")
        T.op("dve", lambda e: e.tensor_copy(out=C.ident_b[:], in_=ident_f[:]), reads=["ident_f"], writes=["ident_b"])
        T.op("pool", lambda e: e.memset(C.ones_b[:], 1.0), writes=["ones_b"])
        T.op("pool", lambda e: e.memset(C.eps_t[:], EPS), writes=["eps_t"])
        with ExitStack() as s0:
            sb0 = lambda n, s, d: sb(n, s, d, s0)
            c_f = sb0("c_f", [128, 16], F32)
            c_s = sb0("c_s", [128, 16], F32)
            c_b = sb0("c_b", [128, 16], BF16)
            crep = sb0("crep", [128, 16, 128], BF16)
            bat = sb0("bat", [128, 96], F32)
            bbc = sb0("bbc", [128, 512], F32)
            gt = sb0("gt", [128, 512], F32)
            was = [sb0("wa%d" % i, [128, 16, 512], BF16) for i in range(2)]
            T.dma("sp", c_f[:], cT, writes=["c_f"], dsem="a0")
            T.dma("sp", bat[:], baT, writes=["bat"], dsem="a1")
            T.op("act", lambda e: e.activation(out=c_s[:], in_=c_f[:], func=AF.Silu), reads=["c_f"], writes=["c_s"])
            T.op("dve", lambda e: e.tensor_copy(out=c_b[:], in_=c_s[:]), reads=["c_s"], writes=["c_b"])
            for kc in range(16):
                T.op("dve", lambda e, kc=kc: e.tensor_scalar(out=crep[:, kc, :], in0=C.ones_b[:], scalar1=c_s[:, kc:kc + 1], scalar2=None, op0=ALU.mult),
                     reads=["c_s", "ones_b"], writes=["crep"])
            for n in range(24):
                wa = was[n % 2]
                wk = "wa%d" % (n % 2)
                T.dma("pool", wa[:], _wview(w_ada[:, n * 512:(n + 1) * 512]), writes=[wk], dsem=wk)
                if (n // 4) in (2, 5):
                    gi = 0 if n // 4 == 2 else 1
                    T.dma("sp", bbc[:], b_ada[0:1, n * 512:(n + 1) * 512].partition_broadcast(128), writes=["bbc"], dsem="a2")
                    for kc in range(16):
                        T.op("pe", lambda e, kc=kc, wa=wa: e.matmul(C.ps_g[:], lhsT=crep[:, kc, :], rhs=wa[:, kc, :], start=(kc == 0), stop=(kc == 15)),
                             reads=[wk, "crep"], writes=["ps_g"], sig=(kc == 15))
                    T.op("dve", lambda e: e.tensor_tensor(out=gt[:], in0=C.ps_g[:], in1=bbc[:], op=ALU.add), reads=["ps_g", "bbc"], writes=["gt"])
                    T.dma("sp", gscr[gi * 128:(gi + 1) * 128, (n % 4) * 512:(n % 4 + 1) * 512], gt[:], reads=["gt"], writes=[("gscr", n)], dsem="a3")
                else:
                    for j in range(4):
                        for kc in range(16):
                            T.op("pe", lambda e, kc=kc, j=j, wa=wa: e.matmul(C.ps_x[:, j:j + 1], lhsT=wa[:, kc, j * 128:(j + 1) * 128], rhs=c_b[:, kc:kc + 1],
                                                                           start=(kc == 0), stop=(kc == 15)),
                                 reads=[wk, "c_b"], writes=["ps_x"], sig=(kc == 15))
                    T.op("dve", lambda e, n=n: e.tensor_tensor(out=modv[:, n * 4:n * 4 + 4], in0=C.ps_x[:, 0:4], in1=bat[:, n * 4:n * 4 + 4], op=ALU.add),
                         reads=["ps_x", "bat"], writes=["modv0"])
            T.barrier()
        T.op("dve", lambda e: e.tensor_scalar(out=AT[:, 0:16], in0=modv[:, 16:32], scalar1=1.0, scalar2=None, op0=ALU.add),
             reads=["modv0"], writes=["AT"])
        T.op("dve", lambda e: e.tensor_scalar(out=AT[:, 16:32], in0=modv[:, 64:80], scalar1=1.0, scalar2=None, op0=ALU.add),
             reads=["modv0"], writes=["AT"])
        T.op("dve", lambda e: e.tensor_tensor(out=AT[:], in0=AT[:], in1=ng[:], op=ALU.mult), reads=["AT", "ng"], writes=["AT"])
        T.barrier()
        A1T = AT[:, 0:16]; A2T = AT[:, 16:32]
        sh1T = modv[:, 0:16]; sh2T = modv[:, 48:64]
        qg = sm[:, 0:1]; kg = sm[:, 1:2]
        convw = lambda g, j: sm[:, 2 + g * 3 + j:3 + g * 3 + j]
        aog = lambda h: sm[:, 26 + h:27 + h]
        cog = lambda g: sm[:, 34 + g:35 + g]
        halo = sm[:, 42:43]

        yT = sb("yT", [128, 8, NT], BF16)
        aT = sb("aT", [128, 8, NT], BF16)
        with ExitStack() as sA:
            sbA = lambda n, s, d: sb(n, s, d, sA)
            QT = sbA("QT", [128, 8, NT], BF16)
            KT = sbA("KT", [128, 8, 2048], BF16)
            V = sbA("V", [128, 16, 1024], BF16)
            with ExitStack() as sP:
                sbP = lambda n, s, d: sb(n, s, d, sP)
                hT = sbP("hT", [128, 16, 2048], BF16)
                with ExitStack() as sX:
                    xb = [sb("xb%d" % i, [128, 2048], F32, sX) for i in range(2)]
                    C.junk = aT[:, 0:2, :].rearrange("p h t -> p (h t)")
                    _xn = sb("xn0", [128, 2048], BF16, sX)
                    C.xn = [_xn, _xn]
                    for i in range(16):
                        src = xp if i < 8 else xo
                        j = i % 8
                        T.dma("sp", xb[i % 2][:], src[j * 128:(j + 1) * 128, :], writes=["xb%d" % (i % 2)], dsem="x%d" % (i % 2))
                        norm_transpose(T, C, xb[i % 2][:], "xb%d" % (i % 2), i, A1T, sh1T, hT, i * 128, "hT")
                    T.barrier()
                wtA = aT[:].rearrange("p h t -> p (h t)").rearrange("p (kc n) -> p kc n", n=512)
                wtB = yT[:].rearrange("p h t -> p (h t)").rearrange("p (kc n) -> p kc n", n=512)
                uz = sbP("uz", [128, 8, NT + 2], BF16)
                zc = sbP("zc", [128, 512], F32)
                sqb2 = sbP("sqb2", [128, 512], BF16)
                rs2 = sbP("rs2", [128, 512], F32)
                C.sqbs = [(C.sqb, "sqb"), (sqb2, "sqb2")]
                C.rss = [(C.rs, "rs"), (rs2, "rs2")]
                C.psns = [(C.ps_n, "ps_n"), (C.ps_d[1], "ps_d1")]
                pb3 = [(C.ps_x, "ps_x"), (C.ps_g, "ps_g"), (C.ps_u, "ps_u")]
                pbi = [0]

                def nbank():
                    r = pb3[pbi[0] % 3]
                    pbi[0] += 1
                    return r

                pend = []

                def flush():
                    while pend:
                        pend.pop(0)()

                for wi, n in enumerate((0, 1, 2, 3, 4, 5, 6, 7, 10, 11)):
                    wt, wtk = (wtA, "wtA") if wi % 2 == 0 else (wtB, "wtB")
                    T.dma("pool", wt, _wview(w_in[:, n * 512:(n + 1) * 512]), writes=[wtk], dsem=wtk)
                    kind = n // 2
                    for hh in range(4):
                        hd = (n % 2) * 4 + hh
                        if kind in (0, 1):
                            chunks = [(1024 + 512 * t, 512 * t) for t in range(2)] if kind == 0 else [(512 * t, 512 * t) for t in range(4)]
                            for (c0, o0) in chunks:
                                px, pxk = nbank()
                                for kc in range(16):
                                    T.op("pe", lambda e, kc=kc, hh=hh, c0=c0, px=px, wt=wt: e.matmul(
                                        px[:], lhsT=wt[:, kc, hh * 128:(hh + 1) * 128], rhs=hT[:, kc, c0:c0 + 512],
                                        start=(kc == 0), stop=(kc == 15)), reads=[wtk], writes=[pxk], sig=(kc == 15))
                                dst = (QT if kind == 0 else KT)[:, hd, o0:o0 + 512]
                                st_ = rms_T_a(T, C, px[:], pxk)
                                flush()
                                pend.append(lambda st_=st_, px=px, pxk=pxk, kind=kind, dst=dst, hd=hd, o0=o0: rms_T_b(
                                    T, C, st_, px[:], pxk, qg if kind == 0 else kg, dst, ("qk", kind, hd, o0), 128))
                        elif kind in (3, 5):
                            for t in range(2):
                                px, pxk = nbank()
                                for kc in range(16):
                                    T.op("pe", lambda e, kc=kc, hh=hh, t=t, px=px, wt=wt: e.matmul(
                                        px[:], lhsT=wt[:, kc, hh * 128:(hh + 1) * 128], rhs=hT[:, kc, 1024 + 512 * t:1536 + 512 * t],
                                        start=(kc == 0), stop=(kc == 15)), reads=[wtk], writes=[pxk], sig=(kc == 15))
                                flush()
                                sl = uz[:, hd, 2 + 512 * t:2 + 512 * (t + 1)]
                                if kind == 3:
                                    T.op("act", lambda e, sl=sl, px=px: e.activation(out=sl, in_=px[:], func=AF.Copy),
                                         reads=[pxk], writes=[("uz", hd, t)])
                                else:
                                    T.op("dve", lambda e, sl=sl, px=px: e.tensor_tensor(out=sl, in0=px[:], in1=sl, op=ALU.mult),
                                         reads=[pxk, ("uz", hd, t)], writes=[("uz", hd, t)])
                            px, pxk = nbank()
                            for kc in range(16):
                                T.op("pe", lambda e, kc=kc, hh=hh, px=px, wt=wt: e.matmul(
                                    px[:, 0:2], lhsT=wt[:, kc, hh * 128:(hh + 1) * 128], rhs=hT[:, kc, 1022:1024],
                                    start=(kc == 0), stop=(kc == 15)), reads=[wtk], writes=[pxk], sig=(kc == 15))
                            sl = uz[:, hd, 0:2]
                            if kind == 3:
                                T.op("act", lambda e, sl=sl, px=px: e.activation(out=sl, in_=px[:, 0:2], func=AF.Copy),
                                     reads=[pxk], writes=[("uz", hd, "h")])
                            else:
                                T.op("dve", lambda e, sl=sl, px=px: e.scalar_tensor_tensor(out=sl, in0=px[:, 0:2], scalar=halo, in1=sl,
                                                                                         op0=ALU.mult, op1=ALU.mult),
                                     reads=[pxk, ("uz", hd, "h")], writes=[("uz", hd, "h")])
                    if kind == 2:
                        for tt in range(16):
                            px, pxk = nbank()
                            for kc in range(16):
                                T.op("pe", lambda e, kc=kc, tt=tt, px=px, wt=wt: e.matmul(
                                    px[:], lhsT=hT[:, kc, tt * 128:(tt + 1) * 128], rhs=wt[:, kc, :],
                                    start=(kc == 0), stop=(kc == 15)), reads=[wtk], writes=[pxk], sig=(kc == 15))
                            flush()
                            T.op("act", lambda e, tt=tt, n=n, px=px: e.activation(out=V[:, tt, (n % 2) * 512:(n % 2 + 1) * 512], in_=px[:], func=AF.Copy),
                                 reads=[pxk], writes=[("V", tt, n)])
                flush()
                T.barrier()
                wt = wtA
                for n in (8, 9):
                    T.dma("pool", wt, _wview(w_in[:, n * 512:(n + 1) * 512]), writes=["wt"], dsem="wt")
                    for hh in range(4):
                        g = (n % 2) * 4 + hh
                        for t in range(2):
                            for kc in range(16):
                                T.op("pe", lambda e, kc=kc, hh=hh, t=t: e.matmul(
                                    C.ps_x[:], lhsT=wt[:, kc, hh * 128:(hh + 1) * 128], rhs=hT[:, kc, 1024 + 512 * t:1536 + 512 * t],
                                    start=(kc == 0), stop=(kc == 15)), reads=["wt"], writes=["ps_x"], sig=(kc == 15))
                            z0, z1, z2 = [uz[:, g, 2 + 512 * t - sh:2 + 512 * (t + 1) - sh] for sh in range(3)]
                            cz = zc[:, 0:512]
                            T.op("dve", lambda e, g=g, z0=z0: e.tensor_scalar(out=cz, in0=z0, scalar1=convw(g, 2), scalar2=None, op0=ALU.mult),
                                 reads=["uzall"], writes=["zc"])
                            T.op("dve", lambda e, g=g, z1=z1: e.scalar_tensor_tensor(out=cz, in0=z1, scalar=convw(g, 1), in1=cz, op0=ALU.mult, op1=ALU.add),
                                 reads=["zc"], writes=["zc"])
                            T.op("dve", lambda e, g=g, z2=z2: e.scalar_tensor_tensor(out=cz, in0=z2, scalar=convw(g, 0), in1=cz, op0=ALU.mult, op1=ALU.add),
                                 reads=["zc"], writes=["zc"])
                            T.op("dve", lambda e: e.tensor_tensor(out=cz, in0=C.ps_x[:], in1=cz, op=ALU.mult), reads=["ps_x", "zc"], writes=["zc"])
                            rms_T(T, C, cz, "zc", cog(g), yT[:, g, 512 * t:512 * (t + 1)], ("yT", g, t), 128)
                T.barrier()
                C.sqbs = None; C.rss = None; C.psns = None
            with ExitStack() as s3:
                sb3 = lambda n, s, d: sb(n, s, d, s3)
                kmf = sb3("kmf", [128, 64], F32)
                kmb = sb3("kmb", [128, 64], BF16)
                gbt = sb3("gbt", [128, 4 * 64], F32)
                es_f = sb3("es_f", [64, 2048], F32)
                es_b = sb3("es_b", [64, 64 * 128], BF16)
                cm_f = sb3("cm_f", [128, 2048], F32)
                cm_b = sb3("cm_b", [128, 2048], BF16)
                gb = sb3("gb", [128, 64], F32)
                mx = sb3("mx", [128, 64], F32)
                sel = sb3("sel", [128, 64], F32)
                val = sb3("val", [128, 64], F32)
                biasT = sb3("biasT", [64, NT], BF16)
                PT = [sb3("PT%d" % i, [128, 512], BF16) for i in range(3)]
                rden = sb3("rden", [128, 512], F32)
                a_f = sb3("a_f", [128, 512], F32)
                T.dma("sp", gbt[:], gbias, writes=["gbt"], dsem="c1a")
                for q4 in range(4):
                    T.dma("sp", es_f[:], esel[:, q4 * 2048:(q4 + 1) * 2048], writes=["es_f"], dsem="c1b")
                    T.op("act", lambda e, q4=q4: e.activation(out=es_b[:, q4 * 2048:(q4 + 1) * 2048], in_=es_f[:], func=AF.Copy),
                         reads=["es_f"], writes=[("es_b", q4)])
                T.dma("sp", cm_f[:], cmask, writes=["cm_f"], dsem="c1c")
                T.op("act", lambda e: e.activation(out=cm_b[:], in_=cm_f[:], func=AF.Copy), reads=["cm_f"], writes=["cm_b"])
                for hd in range(8):
                    T.op("dve", lambda e, hd=hd: e.tensor_reduce(out=kmf[:, hd * 8:(hd + 1) * 8],
                                                                 in_=KT[:, hd, :].rearrange("p (n k) -> p n k", k=256),
                                                                 axis=AX.X, op=ALU.add), writes=["kmf"])
                T.op("dve", lambda e: e.tensor_scalar(out=kmb[:], in0=kmf[:], scalar1=1.0 / 256, scalar2=None, op0=ALU.mult),
                     reads=["kmf"], writes=["kmb"])
                for qt in range(8):
                    j = 4 + qt // 2
                    for hd in range(8):
                        T.op("pe", lambda e, hd=hd, qt=qt: e.matmul(C.ps_x[:, hd * 8:(hd + 1) * 8], lhsT=QT[:, hd, qt * 128:(qt + 1) * 128],
                                                                    rhs=kmb[:, hd * 8:(hd + 1) * 8], start=True, stop=True),
                             reads=["kmb"], writes=["ps_x"], sig=(hd == 7))
                    T.op("dve", lambda e, qt=qt: e.tensor_tensor(out=gb[:], in0=C.ps_x[:, 0:64], in1=gbt[:, (qt // 2) * 64:(qt // 2 + 1) * 64], op=ALU.add),
                         reads=["ps_x", "gbt"], writes=["gb"])
                    for hd in range(8):
                        T.op("dve", lambda e, hd=hd: e.max(out=mx[:, hd * 8:(hd + 1) * 8], in_=gb[:, hd * 8:(hd + 1) * 8]),
                             reads=["gb"], writes=[("mx", hd)])
                        T.op("dve", lambda e, hd=hd: e.tensor_scalar(out=sel[:, hd * 8:(hd + 1) * 8], in0=gb[:, hd * 8:(hd + 1) * 8],
                                                                     scalar1=mx[:, hd * 8 + 2:hd * 8 + 3], scalar2=None, op0=ALU.is_ge),
                             reads=["gb", ("mx", hd)], writes=["sel"])
                    T.op("dve", lambda e: e.tensor_scalar(out=val[:], in0=gb[:], scalar1=-1e29, scalar2=None, op0=ALU.is_gt),
                         reads=["gb"], writes=["val"])
                    T.op("dve", lambda e: e.tensor_tensor(out=sel[:], in0=sel[:], in1=val[:], op=ALU.mult),
                         reads=["val", "sel"], writes=["sel"])
                    T.op("dve", lambda e, j=j: e.memset(sel[:].rearrange("p (h n) -> p h n", n=8)[:, :, j:j + 1], 1.0),
                         reads=["sel"], writes=["sel"])
                    T.op("dve", lambda e: e.tensor_scalar(out=sel[:], in0=sel[:], scalar1=-1.0, scalar2=-NEG, op0=ALU.add, op1=ALU.mult),
                         reads=["sel"], writes=["sel"])
                    T.op("pe", lambda e: e.transpose(out=C.ps_n[0:64, 0:128], in_=sel[:], identity=ident_f[:]),
                         reads=["sel"], writes=["ps_n"])
                    T.op("act", lambda e, qt=qt: e.activation(out=biasT[:, qt * 128:(qt + 1) * 128], in_=C.ps_n[0:64, 0:128], func=AF.Copy),
                         reads=["ps_n"], writes=[("biasT", qt)])
                T.barrier()
                steps = [(hd, qc, kt, (12 if qc == 0 else 16)) for hd in range(8) for qc in range(2) for kt in range(12 if qc == 0 else 16)]

                def scores(i):
                    hd, qc, kt, nk = steps[i]
                    r = kt - (8 + qc * 4)
                    diag = 0 <= r < 4
                    ps = C.ps_d[i % 2]; pk = "ps_d%d" % (i % 2)
                    T.op("pe", lambda e, ps=ps, hd=hd, kt=kt, qc=qc: e.matmul(
                        ps[:], lhsT=KT[:, hd, kt * 128:(kt + 1) * 128], rhs=QT[:, hd, qc * 512:(qc + 1) * 512], start=True, stop=False),
                        writes=[pk], sig=False)
                    T.op("pe", lambda e, ps=ps, hd=hd, kt=kt, qc=qc, diag=diag: e.matmul(
                        ps[:], lhsT=es_b[:, (hd * 8 + kt // 2) * 128:(hd * 8 + kt // 2 + 1) * 128], rhs=biasT[:, qc * 512:(qc + 1) * 512],
                        start=False, stop=(not diag)), reads=["es_b"], writes=[pk], sig=(not diag))
                    if diag:
                        T.op("pe", lambda e, ps=ps, r=r: e.matmul(ps[:], lhsT=C.ident_b[:], rhs=cm_b[:, r * 512:(r + 1) * 512],
                                                                  start=False, stop=True), reads=["cm_b"], writes=[pk])

                scores(0)
                for i, (hd, qc, kt, nk) in enumerate(steps):
                    ps = C.ps_d[i % 2]; pk = "ps_d%d" % (i % 2)
                    pt = PT[i % 3]; ptk = "PT%d" % (i % 3)
                    if i + 1 < len(steps):
                        scores(i + 1)
                    T.op("act", lambda e, ps=ps, pt=pt: e.activation(out=pt[:], in_=ps[:], func=AF.Exp, scale=128.0 ** -0.5),
                         reads=[pk], writes=[ptk])
                    T.op("pe", lambda e, pt=pt, hd=hd, kt=kt, nk=nk: e.matmul(
                        C.ps_g[:], lhsT=V[:, kt, hd * 128:(hd + 1) * 128], rhs=pt[:], start=(kt == 0), stop=(kt == nk - 1)),
                        reads=[ptk], writes=["ps_g"], sig=False)
                    T.op("pe", lambda e, pt=pt, kt=kt, nk=nk: e.matmul(
                        C.ps_u[:], lhsT=C.ones_b[:], rhs=pt[:], start=(kt == 0), stop=(kt == nk - 1)),
                        reads=[ptk], writes=["ps_u"])
                    if kt == nk - 1:
                        T.op("dve", lambda e: e.reciprocal(out=rden[:], in_=C.ps_u[:]), reads=["ps_u"], writes=["rden"])
                        T.op("dve", lambda e: e.tensor_tensor(out=a_f[:], in0=C.ps_g[:], in1=rden[:], op=ALU.mult),
                             reads=["ps_g", "rden"], writes=["a_f"])
                        rms_T(T, C, a_f[:], "a_f", aog(hd), aT[:, hd, qc * 512:(qc + 1) * 512], ("aT", hd, qc), 128)
                T.barrier()
        if debug:
            o_a = nc.dram_tensor("o_a", [128, 8 * NT], BF16, kind="ExternalOutput").ap()
            o_y = nc.dram_tensor("o_y", [128, 8 * NT], BF16, kind="ExternalOutput").ap()
            T.dma("sp", o_a, aT[:].rearrange("p h t -> p (h t)"), writes=["o_a"], dsem="dbg0")
            T.dma("sp", o_y, yT[:].rearrange("p h t -> p (h t)"), writes=["o_y"], dsem="dbg1")
            T.barrier()
        x1 = sbr("x1", [128, 8, D], F32)
        hfT = sbr("hfT", [128, 16, NT], BF16)
        rw = sbr("rw", [128, 8, 256], F32)
        g2bc = sbr("g2bc", [128, D], F32)
        with ExitStack() as s4:
            g1bc = sb("g1bc", [128, D], F32, s4)
            wo = sb("wo", [128, 16, 512], BF16, s4)
            tmp = sb("tmp4", [128, 512], F32, s4)
            C.junk = sb("junk4", [128, 2048], BF16, s4)[:]
            C.xn = [sb("xn4_%d" % i, [128, 2048], BF16, s4) for i in range(2)]
            T.dma("sp", g1bc[:], gscr[0:128, :], writes=["g1bc"], dsem="c2")
            for tt in range(8):
                T.dma("sp", x1[:, tt, :], xo[tt * 128:(tt + 1) * 128, :], writes=[("x1", tt)], dsem="x1_%d" % tt)
            for n in range(4):
                T.dma("pool", wo[:], _wview(w_o[:, n * 512:(n + 1) * 512]), writes=["wo"], dsem="wt")
                for tt in range(8):
                    ps = C.ps_d[tt % 2]; pk = "ps_d%d" % (tt % 2)
                    for kc in range(16):
                        T.op("pe", lambda e, ps=ps, kc=kc, tt=tt: e.matmul(ps[:], lhsT=(aT if kc < 8 else yT)[:, kc % 8, tt * 128:(tt + 1) * 128], rhs=wo[:, kc, :],
                                                                         start=(kc == 0), stop=(kc == 15)),
                             reads=["wo"], writes=[pk], sig=(kc == 15))
                    T.op("dve", lambda e, ps=ps, n=n: e.tensor_tensor(out=tmp[:], in0=ps[:], in1=g1bc[:, n * 512:(n + 1) * 512], op=ALU.mult),
                         reads=[pk, "g1bc"], writes=["tmp4"])
                    T.op("dve", lambda e, tt=tt, n=n: e.tensor_tensor(out=x1[:, tt, n * 512:(n + 1) * 512], in0=x1[:, tt, n * 512:(n + 1) * 512],
                                                                     in1=tmp[:], op=ALU.add),
                         reads=["tmp4", ("x1", tt)], writes=[("x1", tt)])
            T.barrier()
            for tt in range(8):
                norm_transpose(T, C, x1[:, tt, :], ("x1", tt), 16 + tt, A2T, sh2T, hfT, tt * 128, "hfT")
            T.barrier()
        with ExitStack() as s5:
            sb5 = lambda n, s, d: sb(n, s, d, s5)
            wr = sb5("wr", [128, 16, 256], BF16)
            rb = sb5("rb", [128, 256], F32)
            sc = sb5("sc", [128, 256], F32)
            ch = sb5("ch", [128, 256], F32)
            mc = sb5("mc", [128, 256], F32)
            m8 = sb5("m8", [128, 64], F32)
            gs = sb5("gs", [128, 8], F32)
            gm = sb5("gm", [128, 8], F32)
            gk = sb5("gk", [128, 8], F32)
            t1 = sb5("t1", [128, 8], F32)
            t8 = sb5("t8", [128, 8], F32)
            den = sb5("den", [128, 1], F32)
            T.dma("pool", wr[:], _wview(w_r), writes=["wr"], dsem="wt")
            T.dma("sp", rb[:], rbias.partition_broadcast(128), writes=["rb"], dsem="c3")
            for tt in range(8):
                for kc in range(16):
                    T.op("pe", lambda e, kc=kc, tt=tt: e.matmul(C.ps_x[:, 0:256], lhsT=hfT[:, kc, tt * 128:(tt + 1) * 128], rhs=wr[:, kc, :],
                                                                start=(kc == 0), stop=(kc == 15)), reads=["wr"], writes=["ps_x"], sig=(kc == 15))
                T.op("act", lambda e: e.activation(out=sc[:], in_=C.ps_x[:, 0:256], func=AF.Sigmoid), reads=["ps_x"], writes=["sc"])
                T.op("dve", lambda e: e.tensor_tensor(out=ch[:], in0=sc[:], in1=rb[:], op=ALU.add), reads=["sc", "rb"], writes=["ch"])
                for g in range(8):
                    T.op("dve", lambda e, g=g: e.max(out=m8[:, g * 8:(g + 1) * 8], in_=ch[:, g * 32:(g + 1) * 32]), reads=["ch"], writes=[("m8", g)])
                m8v = m8[:].rearrange("p (g k) -> p g k", k=8)
                T.op("dve", lambda e: e.tensor_tensor(out=gs[:], in0=m8v[:, :, 0], in1=m8v[:, :, 1], op=ALU.add),
                     reads=[("m8", g) for g in range(8)], writes=["gs"])
                T.op("dve", lambda e: e.max(out=gm[:], in_=gs[:]), reads=["gs"], writes=["gm"])
                T.op("dve", lambda e: e.tensor_scalar(out=gk[:], in0=gs[:], scalar1=gm[:, 3:4], scalar2=None, op0=ALU.is_ge),
                     reads=["gs", "gm"], writes=["gk"])
                T.op("dve", lambda e: e.tensor_scalar(out=t1[:], in0=gk[:], scalar1=-1.0, scalar2=1e30, op0=ALU.add, op1=ALU.mult),
                     reads=["gk"], writes=["t1"])
                for g in range(8):
                    T.op("dve", lambda e, g=g: e.tensor_scalar(out=mc[:, g * 32:(g + 1) * 32], in0=ch[:, g * 32:(g + 1) * 32],
                                                               scalar1=gk[:, g:g + 1], scalar2=t1[:, g:g + 1], op0=ALU.mult, op1=ALU.add),
                         reads=["ch", "gk", "t1"], writes=["mc"])
                T.op("dve", lambda e: e.max(out=t8[:], in_=mc[:]), reads=["mc"], writes=["t8"])
                T.op("dve", lambda e: e.tensor_scalar(out=mc[:], in0=mc[:], scalar1=t8[:, 7:8], scalar2=None, op0=ALU.is_ge),
                     reads=["t8", "mc"], writes=["mc"])
                T.op("dve", lambda e: e.tensor_tensor(out=mc[:], in0=mc[:], in1=sc[:], op=ALU.mult), reads=["mc", "sc"], writes=["mc"])
                T.op("dve", lambda e: e.tensor_reduce(out=den[:], in_=mc[:], axis=AX.X, op=ALU.add), reads=["mc"], writes=["den"])
                T.op("dve", lambda e: e.reciprocal(out=den[:], in_=den[:]), reads=["den"], writes=["den"])
                T.op("dve", lambda e, tt=tt: e.tensor_scalar(out=rw[:, tt, :], in0=mc[:], scalar1=den[:, 0:1], scalar2=2.5, op0=ALU.mult, op1=ALU.mult),
                     reads=["mc", "den"], writes=[("rw", tt)])
            T.barrier()
        with ExitStack() as s6:
            sb6 = lambda n, s, d: sb(n, s, d, s6)
            rA = sb6("ringA", [128, 8192], BF16)
            rB = sb6("ringB", [128, 8192], BF16)
            ring = [aT[:].rearrange("p h t -> p (h t)"), yT[:].rearrange("p h t -> p (h t)"), rA[:], rB[:]]
            onec = sb6("onec", [128, 1], F32)
            C.hid = sb6("hid", [128, 4, NT], BF16)
            T.op("pool", lambda e: e.memset(onec[:], 1.0), writes=["onec"])
            T.dma("sp", g2bc[:], gscr[128:256, :], writes=["g2bc"], dsem="c4")
            rcnt = [0]
            pbanks = [(C.ps_d[0], "ps_d0"), (C.ps_d[1], "ps_d1"), (C.ps_n, "ps_n"), (C.ps_x, "ps_x")]
            pcnt6 = [0]

            def load_w(src2d, nk):
                i = rcnt[0] % 4
                rcnt[0] += 1
                v = ring[i].rearrange("p (kc n) -> p kc n", n=(512 if nk == 16 else D))
                T.dma("pool", v, _wview(src2d), writes=["ring%d" % i], dsem="ring%d" % i)
                return v, "ring%d" % i

            for ex in list(range(ne)) + [256]:
                if ex < 256:
                    wg, wgk = load_w(wg_d[ex * D:(ex + 1) * D, :], 16)
                    wu, wuk = load_w(wu_d[ex * D:(ex + 1) * D, :], 16)
                    wd, wdk = load_w(wd_d[ex * 512:(ex + 1) * 512, :], 4)
                    scal_of_tt = lambda tt, ex=ex: rw[:, tt, ex:ex + 1]
                    scal_key = ("rw", "all")
                else:
                    wg, wgk = load_w(wsg, 16)
                    wu, wuk = load_w(wsu, 16)
                    wd, wdk = load_w(wsd, 4)
                    scal_of_tt = lambda tt: onec[:, 0:1]
                    scal_key = "onec"
                expert_dense2(T, C, hfT, "hfT", wg, wgk, wu, wuk, wd, wdk, g2bc, scal_of_tt, scal_key, x1, pbanks, pcnt6)
            for tt in range(8):
                T.dma("sp", y_out[tt * 128:(tt + 1) * 128, :], x1[:, tt, :], reads=[("x1", tt, n) for n in range(4)],
                      writes=[("y", tt)], dsem="o2")
        T.emit()
    return nc


def build_l2():
    nc = bass.Bass("TRN2", target_bir_lowering=False)
    hfa = nc.dram_tensor("hfa", [NCORES * D, NT], BF16, kind="ExternalInput").ap()
    rwl = nc.dram_tensor("rwl", [NCORES * NT, 32], F32, kind="ExternalInput").ap()
    wg_d = nc.dram_tensor("wg", [32 * D, 512], F32, kind="ExternalInput").ap()
    wu_d = nc.dram_tensor("wu", [32 * D, 512], F32, kind="ExternalInput").ap()
    wd_d = nc.dram_tensor("wd", [32 * 512, D], F32, kind="ExternalInput").ap()
    part = nc.dram_tensor("part", [NCORES * NT, D], F32, kind="ExternalOutput").ap()
    with ExitStack() as st:
        T = Tracker(nc, st)
        C = Ctx()
        sb = lambda n, s, d: st.enter_context(nc.sbuf_tensor(n, s, d))
        psf = lambda n: st.enter_context(nc.psum_tensor(n, [128, 512], F32))
        C.ps_g = psf("ps_g"); C.ps_u = psf("ps_u")
        C.ps_d = [psf("ps_d0"), psf("ps_d1")]
        C.sg = sb("sg", [128, 512], F32)
        C.hid = sb("hid", [128, 4, NT], BF16)
        hT = sb("hT", [128, 16, NT], BF16)
        acc = sb("acc", [128, 8, D], F32)
        rw = sb("rw", [128, 8, 32], F32)
        ring = [sb("ring%d" % i, [128, 8192], BF16) for i in range(4)]
        rcnt = [0]

        def load_w(src2d, shape3):
            i = rcnt[0] % 4
            rcnt[0] += 1
            t = ring[i]
            if shape3 == 16:
                v = t[:].rearrange("p (kc n) -> p kc n", n=512)
            else:
                v = t[:].rearrange("p (kc n) -> p kc n", n=D)
            T.dma("pool", v, _wview(src2d), writes=["ring%d" % i], dsem="ring%d" % i)
            return v, "ring%d" % i

        for r in range(NCORES):
            T.dma("sp", hT[:], hfa[r * D:(r + 1) * D, :].rearrange("(kc p) t -> p kc t", p=128), writes=["hT"], dsem="h")
            T.dma("sp", rw[:], rwl[r * NT:(r + 1) * NT, :].rearrange("(tt p) e -> p tt e", p=128), writes=["rw"], dsem="hr")
            for ex in range(32):
                wg, wgk = load_w(wg_d[ex * D:(ex + 1) * D, :], 16)
                wu, wuk = load_w(wu_d[ex * D:(ex + 1) * D, :], 16)
                wd, wdk = load_w(wd_d[ex * 512:(ex + 1) * 512, :], 4)

                def epi(tt, n, ps, pk, ex=ex):
                    a = acc[:, tt, n * 512:(n + 1) * 512]
                    if ex == 0:
                        T.op("dve", lambda e: e.tensor_scalar(out=a, in0=ps[:], scalar1=rw[:, tt, ex:ex + 1], scalar2=None, op0=ALU.mult),
                             reads=[pk, "rw"], writes=[("acc", tt, n)])
                    else:
                        T.op("dve", lambda e: e.scalar_tensor_tensor(out=a, in0=ps[:], scalar=rw[:, tt, ex:ex + 1], in1=a, op0=ALU.mult, op1=ALU.add),
                             reads=[pk, "rw"], writes=[("acc", tt, n)])

                expert_dense(T, C, hT, "hT", wg, wgk, wu, wuk, wd, wdk, epi)
            for tt in range(8):
                T.dma("sp", part[r * NT + tt * 128:r * NT + (tt + 1) * 128, :], acc[:, tt, :],
                      reads=[("acc", tt, n) for n in range(4)], writes=[("part", r, tt)], dsem="o%d" % tt)
        T.emit()
    return nc


def build_l3():
    nc = bass.Bass("TRN2", target_bir_lowering=False)
    parts = nc.dram_tensor("parts", [NCORES * NT, D], F32, kind="ExternalInput").ap()
    x1s = nc.dram_tensor("x1s", [NT, D], F32, kind="ExternalInput").ap()
    g2row = nc.dram_tensor("g2row", [1, D], F32, kind="ExternalInput").ap()
    y = nc.dram_tensor("y", [NT, D], F32, kind="ExternalOutput").ap()
    with ExitStack() as st:
        T = Tracker(nc, st)
        sb = lambda n, s, d: st.enter_context(nc.sbuf_tensor(n, s, d))
        g2bc = sb("g2bc", [128, D], F32)
        pb = [sb("pb%d" % i, [128, 8, D], F32) for i in range(2)]
        xb = [sb("xb%d" % i, [128, D], F32) for i in range(2)]
        T.dma("sp", g2bc[:], g2row.partition_broadcast(128), writes=["g2bc"], dsem="c")
        for tt in range(8):
            b = tt % 2
            T.dma("sp", xb[b][:], x1s[tt * 128:(tt + 1) * 128, :], writes=["xb%d" % b], dsem="q%d" % b)
            for c in range(NCORES):
                T.dma("sp", pb[b][:, c, :], parts[c * NT + tt * 128:c * NT + (tt + 1) * 128, :], writes=["pb%d" % b], dsem="p%d" % b)
            for c in range(1, NCORES):
                T.op("dve", lambda e, b=b, c=c: e.tensor_tensor(out=pb[b][:, 0, :], in0=pb[b][:, 0, :], in1=pb[b][:, c, :], op=ALU.add),
                     reads=["pb%d" % b], writes=["pb%d" % b])
            T.op("dve", lambda e, b=b: e.tensor_tensor(out=pb[b][:, 0, :], in0=pb[b][:, 0, :], in1=g2bc[:], op=ALU.mult),
                 reads=["pb%d" % b, "g2bc"], writes=["pb%d" % b])
            T.op("dve", lambda e, b=b: e.tensor_tensor(out=xb[b][:], in0=xb[b][:], in1=pb[b][:, 0, :], op=ALU.add),
                 reads=["pb%d" % b, "xb%d" % b], writes=["xb%d" % b])
            T.dma("sp", y[tt * 128:(tt + 1) * 128, :], xb[b][:], reads=["xb%d" % b], writes=[("y", tt)], dsem="o%d" % b)
        T.emit()
    return nc


def _consts(h):
    ident = np.eye(128, dtype=np.float32)
    gb = np.zeros((128, 4, 8, 8), np.float32)
    for jj in range(4):
        j = 4 + jj
        gb[:, jj, :, j:] = -1e30
        if h == 0:
            gb[:, jj, :, 0:4] = -1e30
    es = np.zeros((64, 64, 128), np.float32)
    for r in range(64):
        es[r, r, :] = 1.0
    cm = np.zeros((128, 4, 512), np.float32)
    k = np.arange(128)[:, None]
    q = np.arange(512)[None, :]
    for r in range(4):
        same = (q // 256) == (r // 2)
        cm[:, r, :] = np.where(same & ((r * 128 + k) > q), NEG, 0.0)
    return ident, gb.reshape(128, 256), es.reshape(64, 8192), cm.reshape(128, 2048)


def _pp(v):
    return np.ascontiguousarray(np.asarray(v, np.float32).reshape(-1, 128).T)


def kernel_unfused(x, c, w_ada, b_ada, norm1_g, w_in, q_norm_g, k_norm_g, conv_w, attn_out_g, conv_out_g, w_o,
           norm2_g, w_router, router_bias, w_gate, w_up, w_down, ws_gate, ws_up, ws_down):
    f = lambda a: np.ascontiguousarray(np.asarray(a, dtype=np.float32))
    x = f(x); c = f(c)
    cores = list(range(NCORES))
    cT = np.ascontiguousarray(c.reshape(4, 16, 128).transpose(2, 1, 0).reshape(128, 64))
    wa = f(w_ada)[0]
    ba = f(b_ada)
    in0 = [{"cT": cT, "wa": np.ascontiguousarray(wa[:, k * 1536:(k + 1) * 1536]),
            "ba": np.ascontiguousarray(ba[:, k * 1536:(k + 1) * 1536])} for k in cores]
    r0 = run_bass_kernel_spmd(build_l0(), in0, core_ids=cores)
    mod = np.concatenate([r0.results[k]["mod"] for k in cores], axis=1)
    w_in0 = f(w_in)[0]; w_o0 = f(w_o)[0]; w_r0 = f(w_router)[0]
    wsg = f(ws_gate)[0]; wsu = f(ws_up)[0]; wsd = f(ws_down)[0]
    ngT = np.concatenate([_pp(f(norm1_g)[0]), _pp(f(norm2_g)[0])], axis=1)
    cw = f(conv_w)[0]
    in1 = []
    for k in cores:
        b, h = k // 2, k % 2
        ident, gb, es, cm = _consts(h)
        small = np.zeros((128, 64), np.float32)
        small[:, 0] = f(q_norm_g)[0]
        small[:, 1] = f(k_norm_g)[0]
        small[:, 2:26] = cw.reshape(3, 8, 128).transpose(2, 1, 0).reshape(128, 24)
        small[:, 26:34] = _pp(f(attn_out_g)[0])
        small[:, 34:42] = _pp(f(conv_out_g)[0])
        small[:, 42] = float(h)
        in1.append({
            "xo": np.ascontiguousarray(x[b, h * NT:(h + 1) * NT]), "xp": np.ascontiguousarray(x[b, 0:NT]),
            "modT": _pp(mod[b]), "g1row": np.ascontiguousarray(mod[b:b + 1, 2 * D:3 * D]),
            "g2row": np.ascontiguousarray(mod[b:b + 1, 5 * D:6 * D]), "ngT": ngT, "w_in": w_in0, "smallT": small,
            "w_o": w_o0, "w_r": w_r0, "rbias": f(router_bias), "wsg": wsg, "wsu": wsu, "wsd": wsd,
            "ident": ident, "gbias": gb, "esel": es, "cmask": cm})
    r1 = run_bass_kernel_spmd(build_l1(), in1, core_ids=cores)
    hfa = np.concatenate([r1.results[k]["o_hfT"] for k in cores], axis=0)
    rw_all = np.concatenate([r1.results[k]["o_rw"] for k in cores], axis=0)
    wg = f(w_gate)[0]; wu = f(w_up)[0]; wd = f(w_down)[0]
    in2 = [{"hfa": hfa, "rwl": np.ascontiguousarray(rw_all[:, 32 * k:32 * (k + 1)]),
            "wg": wg[32 * k:32 * (k + 1)].reshape(32 * D, 512), "wu": wu[32 * k:32 * (k + 1)].reshape(32 * D, 512),
            "wd": wd[32 * k:32 * (k + 1)].reshape(32 * 512, D)} for k in cores]
    r2 = run_bass_kernel_spmd(build_l2(), in2, core_ids=cores)
    in3 = []
    for k in cores:
        b = k // 2
        parts = np.concatenate([r2.results[cc]["part"][k * NT:(k + 1) * NT] for cc in cores], axis=0)
        in3.append({"parts": parts, "x1s": r1.results[k]["o_x1s"], "g2row": np.ascontiguousarray(mod[b:b + 1, 5 * D:6 * D])})
    r3 = run_bass_kernel_spmd(build_l3(), in3, core_ids=cores)
    out = np.stack([np.concatenate([r3.results[2 * b]["y"], r3.results[2 * b + 1]["y"]], axis=0) for b in range(NB)], axis=0)
    return out.astype(np.float32)


def fused_inputs(x, c, w_ada, b_ada, norm1_g, w_in, q_norm_g, k_norm_g, conv_w, attn_out_g, conv_out_g, w_o,
                 norm2_g, w_router, router_bias, w_gate, w_up, w_down, ws_gate, ws_up, ws_down, cores, ne=256):
    f = lambda a: np.ascontiguousarray(np.asarray(a, dtype=np.float32))
    x = f(x); c = f(c)
    wa = f(w_ada)[0]; ba = f(b_ada)
    w_in0 = f(w_in)[0]; w_o0 = f(w_o)[0]; w_r0 = f(w_router)[0]
    wsg = f(ws_gate)[0]; wsu = f(ws_up)[0]; wsd = f(ws_down)[0]
    nex = max(ne, 1)
    wg = f(w_gate)[0][:nex].reshape(nex * D, 512)
    wu = f(w_up)[0][:nex].reshape(nex * D, 512)
    wd = f(w_down)[0][:nex].reshape(nex * 512, D)
    ngT = np.concatenate([_pp(f(norm1_g)[0]), _pp(f(norm2_g)[0])], axis=1)
    cw = f(conv_w)[0]
    baT = _pp(ba[0])
    rb = f(router_bias)
    ins = []
    for k in cores:
        b, h = k // 2, k % 2
        ident, gb, es, cm = _consts(h)
        small = np.zeros((128, 64), np.float32)
        small[:, 0] = f(q_norm_g)[0]
        small[:, 1] = f(k_norm_g)[0]
        small[:, 2:26] = cw.reshape(3, 8, 128).transpose(2, 1, 0).reshape(128, 24)
        small[:, 26:34] = _pp(f(attn_out_g)[0])
        small[:, 34:42] = _pp(f(conv_out_g)[0])
        small[:, 42] = float(h)
        ins.append({
            "xo": np.ascontiguousarray(x[b, h * NT:(h + 1) * NT]), "xp": np.ascontiguousarray(x[b, 0:NT]),
            "cT": _pp(c[b]), "w_ada": wa, "baT": baT, "b_ada": ba, "ngT": ngT, "w_in": w_in0, "smallT": small,
            "w_o": w_o0, "w_r": w_r0, "rbias": rb, "wsg": wsg, "wsu": wsu, "wsd": wsd,
            "ident": ident, "gbias": gb, "esel": es, "cmask": cm, "wg": wg, "wu": wu, "wd": wd})
    return ins


def kernel(**inputs):
    cores = list(range(NCORES))
    ins = fused_inputs(cores=cores, **inputs)
    res = run_bass_kernel_spmd(build_fused(), ins, core_ids=cores)
    out = np.stack([np.concatenate([res.results[2 * b]["y"], res.results[2 * b + 1]["y"]], axis=0) for b in range(NB)], axis=0)
    return out.astype(np.float32)
```
